# Optimizing a Trainium2 kernel written in Bass

```python
import jax
import jax.numpy as jnp
from jax import lax
import numpy as np

D_MODEL = 1024
BATCH = 16
SEQ = 2048
DEPTH = 2

GRID_W = 64
CTX_LEN = 256
HEAD_DIM = 64
N_MIXERS = 4
GROUP_W = D_MODEL // N_MIXERS
MIX_W = N_MIXERS * GROUP_W
A_HEADS = GROUP_W // HEAD_DIM
A_KV_HEADS = A_HEADS // 2
WINDOW = 128
WIN_BLOCK = 128
B_HEADS = GROUP_W // HEAD_DIM
NB_ROWS = 8
NB_COLS = 16
NB_KEY_COLS = 2 * NB_COLS
CONV_W = 3
D_HEADS = GROUP_W // HEAD_DIM
SCAN_CHUNK = 64
LB_FLOOR = 1e-20
ROPE_THETA = 10000.0
AXIS_DIM = HEAD_DIM // 2
PEER_HEADS = 8
PEER_NKEYS = 128
PEER_EXPERTS = PEER_NKEYS * PEER_NKEYS
PEER_DQ = D_MODEL // 4
PEER_TOPK = 16
PEER_TOKEN_BLOCK = 128
N_MOD = 6
EPS = 1e-6
MASK_VALUE = -1e30

SPLIT_SIZES = (A_HEADS * HEAD_DIM, A_KV_HEADS * HEAD_DIM, A_KV_HEADS * HEAD_DIM,
               B_HEADS * HEAD_DIM, B_HEADS * HEAD_DIM, B_HEADS * HEAD_DIM,
               GROUP_W, GROUP_W, GROUP_W,
               GROUP_W, GROUP_W, GROUP_W, GROUP_W, GROUP_W)
IN_W = sum(SPLIT_SIZES)

kernel_name = 'hybrid_parallel_mixer_dit_peer'


def _rmsnorm(x, g):
    xf = x.astype(jnp.float32)
    y = xf * lax.rsqrt(jnp.mean(xf * xf, axis=-1, keepdims=True) + EPS)
    return (y * g.astype(jnp.float32)).astype(x.dtype)


def _modulation(cvec, w, b):
    m = jax.nn.silu(cvec) @ w + b
    if m.ndim == 2:
        m = m[:, None, :]
    return jnp.split(m, N_MOD, axis=-1)


def _split_proj(p):
    points = np.cumsum(SPLIT_SIZES)[:-1].tolist()
    return jnp.split(p, points, axis=-1)


def _heads(t):
    return t.reshape(t.shape[0], t.shape[1], -1, HEAD_DIM)


def _axial_angles(L):
    t = jnp.arange(L)
    inv = ROPE_THETA ** (-jnp.arange(0, AXIS_DIM, 2, dtype=jnp.float32) / AXIS_DIM)
    row = (t // GRID_W).astype(jnp.float32)[:, None] * inv
    col = (t % GRID_W).astype(jnp.float32)[:, None] * inv
    return row, col


def _rot_half(x, ang):
    x1, x2 = jnp.split(x, 2, axis=-1)
    c = jnp.cos(ang)[None, :, None, :]
    s = jnp.sin(ang)[None, :, None, :]
    return jnp.concatenate([x1 * c - x2 * s, x1 * s + x2 * c], axis=-1)


def _axial_rope(x, row_ang, col_ang):
    xf = x.astype(jnp.float32)
    xr, xc = jnp.split(xf, 2, axis=-1)
    return jnp.concatenate([_rot_half(xr, row_ang), _rot_half(xc, col_ang)], axis=-1).astype(x.dtype)


def _ctx_attention(q, k, v, sink):
    B, C, H, dh = q.shape
    Hk = k.shape[2]
    G = H // Hk
    qg = q.reshape(B, C, Hk, G, dh)
    s = jnp.einsum('bqhgd,bshd->bhgqs', qg, k).astype(jnp.float32) * dh ** -0.5
    if sink is not None:
        s_sink = jnp.broadcast_to(sink.astype(jnp.float32).reshape(Hk, G)[None, :, :, None, None], (B, Hk, G, C, 1))
        s = jnp.concatenate([s, s_sink], axis=-1)
    p = jax.nn.softmax(s, axis=-1)[..., :C].astype(v.dtype)
    o = jnp.einsum('bhgqs,bshd->bqhgd', p, v)
    return o.reshape(B, C, H * dh)


def _window_attention(q, k, v, k_ctx, v_ctx, sink, row_ang, col_ang):
    B, L, H, dh = q.shape
    G = H // A_KV_HEADS
    C = k_ctx.shape[1]
    nb = L // WIN_BLOCK
    span = WIN_BLOCK + 2 * WINDOW
    scale = dh ** -0.5
    q = _axial_rope(q, row_ang, col_ang)
    k = _axial_rope(k, row_ang, col_ang)
    pad = ((0, 0), (WINDOW, WINDOW), (0, 0), (0, 0))
    idx = jnp.arange(nb)[:, None] * WIN_BLOCK + jnp.arange(span)[None, :]
    kb = jnp.pad(k, pad)[:, idx]
    vb = jnp.pad(v, pad)[:, idx]
    qpos = jnp.arange(nb)[:, None] * WIN_BLOCK + jnp.arange(WIN_BLOCK)[None, :]
    kpos = idx - WINDOW
    mask = ((jnp.abs(qpos[:, :, None] - kpos[:, None, :]) <= WINDOW)
            & (kpos[:, None, :] >= 0) & (kpos[:, None, :] < L))
    qb = q.reshape(B, nb, WIN_BLOCK, A_KV_HEADS, G, dh)
    s_loc = jnp.einsum('bnqhgd,bnshd->bnhgqs', qb, kb).astype(jnp.float32) * scale
    s_loc = jnp.where(mask[None, :, None, None], s_loc, MASK_VALUE)
    s_ctx = jnp.einsum('bnqhgd,bchd->bnhgqc', qb, k_ctx).astype(jnp.float32) * scale
    s_sink = jnp.broadcast_to(sink.astype(jnp.float32).reshape(A_KV_HEADS, G)[None, None, :, :, None, None],
                              s_loc.shape[:-1] + (1,))
    p = jax.nn.softmax(jnp.concatenate([s_loc, s_ctx, s_sink], axis=-1), axis=-1)
    p_loc = p[..., :span].astype(v.dtype)
    p_ctx = p[..., span:span + C].astype(v.dtype)
    o = (jnp.einsum('bnhgqs,bnshd->bnqhgd', p_loc, vb)
         + jnp.einsum('bnhgqc,bchd->bnqhgd', p_ctx, v_ctx))
    return o.reshape(B, L, H * dh)


def _neighbourhood_attention(q, k, v, k_ctx, v_ctx, rpb):
    B, L, H, dh = q.shape
    C = k_ctx.shape[1]
    rows = L // GRID_W
    kr = min(NB_ROWS, rows)
    ncb = GRID_W // NB_COLS
    S = kr * NB_KEY_COLS
    scale = dh ** -0.5
    r = jnp.arange(rows)
    row_idx = jnp.clip(r - kr // 2, 0, rows - kr)[:, None] + jnp.arange(kr)[None, :]
    cblk = jnp.arange(ncb)
    col_idx = (jnp.clip(cblk * NB_COLS - NB_COLS // 2, 0, GRID_W - NB_KEY_COLS)[:, None]
               + jnp.arange(NB_KEY_COLS)[None, :])
    qcol = cblk[:, None] * NB_COLS + jnp.arange(NB_COLS)[None, :]
    win0 = jnp.clip(qcol - NB_COLS // 2, 0, GRID_W - NB_COLS)
    kc = col_idx[:, None, :]
    col_ok = (kc >= win0[:, :, None]) & (kc < win0[:, :, None] + NB_COLS)
    mask = jnp.broadcast_to(col_ok[:, :, None, :], (ncb, NB_COLS, kr, NB_KEY_COLS)).reshape(ncb, NB_COLS, S)
    d_row = row_idx - r[:, None]
    d_col = jnp.clip(kc - qcol[:, :, None], -(NB_COLS - 1), NB_COLS - 1)
    bias = rpb[:, d_row[:, None, None, :, None] + (NB_ROWS - 1), d_col[None, :, :, None, :] + (NB_COLS - 1)]
    bias = bias.reshape(H, rows, ncb, NB_COLS, S).transpose(1, 2, 0, 3, 4).astype(jnp.float32)
    kg = k.reshape(B, rows, GRID_W, H, dh)
    vg = v.reshape(B, rows, GRID_W, H, dh)
    ri = row_idx[:, None, :, None]
    ci = col_idx[None, :, None, :]
    kb = kg[:, ri, ci].reshape(B, rows, ncb, S, H, dh)
    vb = vg[:, ri, ci].reshape(B, rows, ncb, S, H, dh)
    qb = q.reshape(B, rows, ncb, NB_COLS, H, dh)
    s_loc = jnp.einsum('brnqhd,brnshd->brnhqs', qb, kb).astype(jnp.float32) * scale + bias[None]
    s_loc = jnp.where(mask[None, None, :, None], s_loc, MASK_VALUE)
    s_ctx = jnp.einsum('brnqhd,bchd->brnhqc', qb, k_ctx).astype(jnp.float32) * scale
    p = jax.nn.softmax(jnp.concatenate([s_loc, s_ctx], axis=-1), axis=-1)
    p_loc = p[..., :S].astype(v.dtype)
    p_ctx = p[..., S:].astype(v.dtype)
    o = (jnp.einsum('brnhqs,brnshd->brnqhd', p_loc, vb)
         + jnp.einsum('brnhqc,bchd->brnqhd', p_ctx, v_ctx))
    return o.reshape(B, L, H * dh)


def _short_conv(u, w):
    ch = u.shape[-1]
    return lax.conv_general_dilated(u, w[:, None, :].astype(u.dtype), window_strides=(1,),
                                    padding=[(CONV_W // 2, CONV_W // 2)],
                                    dimension_numbers=('NWC', 'WIO', 'NWC'),
                                    feature_group_count=ch)


def _hgrn2_gates(z, lb):
    log_lb = jnp.log(jnp.maximum(lb, LB_FLOOR))
    logf = jnp.logaddexp(log_lb, jnp.log1p(-lb) + jax.nn.log_sigmoid(z.astype(jnp.float32)))
    return -jnp.expm1(logf), logf


def _hgrn2_scan(q, k, v, logf, s0):
    B, L, H, _ = q.shape
    dv = v.shape[-1]
    n = L // SCAN_CHUNK

    def chunks(a):
        return a.astype(jnp.float32).reshape(B, n, SCAN_CHUNK, H, a.shape[-1]).transpose(1, 0, 3, 2, 4)

    tril = jnp.tril(jnp.ones((SCAN_CHUNK, SCAN_CHUNK), dtype=bool))[:, :, None]

    def step(S, inp):
        qc, kc, vc, lc = inp
        G = jnp.cumsum(lc, axis=2)
        diff = G[:, :, :, None, :] - G[:, :, None, :, :]
        decay = jnp.where(tril, jnp.exp(jnp.where(tril, diff, 0.0)), 0.0)
        attn = jnp.einsum('bhid,bhjd,bhijd->bhij', qc, kc, decay)
        o = jnp.einsum('bhij,bhjv->bhiv', attn, vc) + jnp.einsum('bhid,bhdv->bhiv', qc * jnp.exp(G), S)
        G_end = G[:, :, -1:, :]
        S = (jnp.exp(G_end[:, :, 0, :, None]) * S
             + jnp.einsum('bhjd,bhjv->bhdv', kc * jnp.exp(G_end - G), vc))
        return S, o

    S, o = lax.scan(step, s0, (chunks(q), chunks(k), chunks(v), chunks(logf)))
    o = o.transpose(1, 0, 3, 2, 4).reshape(B, L, H, dv)
    return o.astype(v.dtype), S


def _hgrn2_bidir(q, v, zf, zb, q_c, v_c, zf_c, zb_c, lb):
    B = q.shape[0]
    lb = lb.reshape(D_HEADS, HEAD_DIM)
    s0 = jnp.zeros((B, D_HEADS, HEAD_DIM, HEAD_DIM), jnp.float32)
    rev = lambda a: a[:, ::-1]
    kf, lf = _hgrn2_gates(zf, lb)
    kf_c, lf_c = _hgrn2_gates(zf_c, lb)
    kb, lbw = _hgrn2_gates(zb, lb)
    kb_c, lbw_c = _hgrn2_gates(zb_c, lb)
    oc_f, s_f = _hgrn2_scan(q_c, kf_c, v_c, lf_c, s0)
    o_f, _ = _hgrn2_scan(q, kf, v, lf, s_f)
    oc_b, s_b = _hgrn2_scan(rev(q_c), rev(kb_c), rev(v_c), rev(lbw_c), s0)
    o_b, _ = _hgrn2_scan(rev(q), rev(kb), rev(v), rev(lbw), s_b)
    return o_f + rev(o_b), oc_f + rev(oc_b)


def _merge_groups(parts, gnorm):
    gains = jnp.split(gnorm, N_MIXERS)
    return jnp.concatenate([_rmsnorm(p, g) for p, g in zip(parts, gains)], axis=-1)


def _mixers(hx, hc, w_in, conv_w, sink, rpb, lb, gnorm, w_out, row_ang, col_ang, with_ctx):
    B, L, _ = hx.shape
    C = hc.shape[1]
    (aq, ak, av, bq, bk, bv, cx, cb, cc, dq, dzf, dzb, di, dg) = _split_proj(hx @ w_in)
    (aq_c, ak_c, av_c, bq_c, bk_c, bv_c, cx_c, cb_c, cc_c, dq_c, dzf_c, dzb_c, di_c, dg_c) = _split_proj(hc @ w_in)
    ak_c, av_c, bk_c, bv_c = _heads(ak_c), _heads(av_c), _heads(bk_c), _heads(bv_c)
    y_a = _window_attention(_heads(aq), _heads(ak), _heads(av), ak_c, av_c, sink, row_ang, col_ang)
    y_b = _neighbourhood_attention(_heads(bq), _heads(bk), _heads(bv), bk_c, bv_c, rpb)
    y_c = cb * _short_conv(cc * cx, conv_w)
    o_d, o_dc = _hgrn2_bidir(_heads(dq), _heads(di), _heads(dzf), _heads(dzb),
                             _heads(dq_c), _heads(di_c), _heads(dzf_c), _heads(dzb_c), lb)
    y_d = o_d.reshape(B, L, GROUP_W) * jax.nn.silu(dg)
    y = _merge_groups((y_a, y_b, y_c, y_d), gnorm) @ w_out
    if not with_ctx:
        return y, None
    yc_a = _ctx_attention(_heads(aq_c), ak_c, av_c, sink)
    yc_b = _ctx_attention(_heads(bq_c), bk_c, bv_c, None)
    yc_c = cb_c * _short_conv(cc_c * cx_c, conv_w)
    yc_d = o_dc.reshape(B, C, GROUP_W) * jax.nn.silu(dg_c)
    yc = _merge_groups((yc_a, yc_b, yc_c, yc_d), gnorm) @ w_out
    return y, yc


def _peer(h, w_q, sub_k1, sub_k2, u, v):
    T, D = h.shape
    q = (h @ w_q).reshape(T, PEER_HEADS, 2, PEER_DQ // 2)
    s1 = jnp.einsum('thd,kd->thk', q[:, :, 0], sub_k1)
    s2 = jnp.einsum('thd,kd->thk', q[:, :, 1], sub_k2)
    v1, i1 = lax.top_k(s1, PEER_TOPK)
    v2, i2 = lax.top_k(s2, PEER_TOPK)
    cand_s = (v1[..., :, None] + v2[..., None, :]).reshape(T, PEER_HEADS, PEER_TOPK * PEER_TOPK)
    cand_i = (i1[..., :, None] * PEER_NKEYS + i2[..., None, :]).reshape(T, PEER_HEADS, PEER_TOPK * PEER_TOPK)
    top_s, pos = lax.top_k(cand_s, PEER_TOPK)
    idx = jnp.take_along_axis(cand_i, pos, axis=-1)
    gate = jax.nn.softmax(top_s.astype(jnp.float32), axis=-1).astype(h.dtype)
    E = PEER_HEADS * PEER_TOPK
    nblk = T // PEER_TOKEN_BLOCK

    def block(args):
        hb, ib, gb = args
        act = jax.nn.gelu(jnp.einsum('td,ted->te', hb, u[ib])) * gb
        return jnp.einsum('te,ted->td', act, v[ib])

    out = lax.map(block, (h.reshape(nblk, PEER_TOKEN_BLOCK, D),
                          idx.reshape(nblk, PEER_TOKEN_BLOCK, E),
                          gate.reshape(nblk, PEER_TOKEN_BLOCK, E)))
    return out.reshape(T, D)


def setup_inputs(seed: int = 0) -> dict:
    key = jax.random.key(seed)
    ks = jax.random.split(key, 21)
    nrm = lambda k, shape, s: jax.random.normal(k, shape, jnp.float32) * s
    return {
        'x': nrm(ks[0], (BATCH, SEQ, D_MODEL), 1.0),
        'c': nrm(ks[1], (BATCH, D_MODEL), 1.0),
        'ctx': nrm(ks[2], (BATCH, CTX_LEN, D_MODEL), 1.0),
        'c_ctx': nrm(ks[3], (D_MODEL,), 1.0),
        'w_mod': nrm(ks[4], (DEPTH, D_MODEL, N_MOD * D_MODEL), 0.5 * D_MODEL ** -0.5),
        'b_mod': nrm(ks[5], (DEPTH, N_MOD * D_MODEL), 0.02),
        'norm_mix': 1.0 + nrm(ks[6], (DEPTH, D_MODEL), 0.02),
        'norm_ffn': 1.0 + nrm(ks[7], (DEPTH, D_MODEL), 0.02),
        'w_in': nrm(ks[8], (DEPTH, D_MODEL, IN_W), D_MODEL ** -0.5),
        'conv_w': nrm(ks[9], (DEPTH, CONV_W, GROUP_W), CONV_W ** -0.5),
        'attn_sink': nrm(ks[10], (DEPTH, A_HEADS), 0.5),
        'na_rpb': nrm(ks[11], (DEPTH, B_HEADS, 2 * NB_ROWS - 1, 2 * NB_COLS - 1), 0.1),
        'lb_logits': nrm(ks[12], (DEPTH, GROUP_W), 0.5),
        'group_norm': 1.0 + nrm(ks[13], (DEPTH, MIX_W), 0.02),
        'w_out': nrm(ks[14], (DEPTH, MIX_W, D_MODEL), MIX_W ** -0.5),
        'peer_wq': nrm(ks[15], (DEPTH, D_MODEL, PEER_HEADS * PEER_DQ), D_MODEL ** -0.5),
        'peer_k1': nrm(ks[16], (DEPTH, PEER_NKEYS, PEER_DQ // 2), (PEER_DQ // 2) ** -0.5),
        'peer_k2': nrm(ks[17], (DEPTH, PEER_NKEYS, PEER_DQ // 2), (PEER_DQ // 2) ** -0.5),
        'peer_u': nrm(ks[18], (DEPTH, PEER_EXPERTS, D_MODEL), D_MODEL ** -0.5),
        'peer_v': nrm(ks[19], (DEPTH, PEER_EXPERTS, D_MODEL), PEER_HEADS ** -0.5),
        'final_norm': 1.0 + nrm(ks[20], (D_MODEL,), 0.02),
    }


def reference(x, c, ctx, c_ctx, w_mod, b_mod, norm_mix, norm_ffn, w_in, conv_w, attn_sink, na_rpb,
              lb_logits, group_norm, w_out, peer_wq, peer_k1, peer_k2, peer_u, peer_v, final_norm):
    B, L, D = x.shape
    C = ctx.shape[1]
    row_ang, col_ang = _axial_angles(L)
    lb_soft = jax.nn.softmax(lb_logits.astype(jnp.float32), axis=0)
    lower_bounds = jnp.cumsum(lb_soft, axis=0) - lb_soft[0:1]
    for l in range(DEPTH):
        last = l == DEPTH - 1
        sh1, sc1, g1, sh2, sc2, g2 = _modulation(c, w_mod[l], b_mod[l])
        sh1c, sc1c, g1c, sh2c, sc2c, g2c = _modulation(c_ctx, w_mod[l], b_mod[l])
        hx = _rmsnorm(x, norm_mix[l]) * (1 + sc1) + sh1
        hc = _rmsnorm(ctx, norm_mix[l]) * (1 + sc1c) + sh1c
        y, yc = _mixers(hx, hc, w_in[l], conv_w[l], attn_sink[l], na_rpb[l], lower_bounds[l],
                        group_norm[l], w_out[l], row_ang, col_ang, not last)
        x = x + g1 * y
        hx2 = _rmsnorm(x, norm_ffn[l]) * (1 + sc2) + sh2
        peer_args = (peer_wq[l], peer_k1[l], peer_k2[l], peer_u[l], peer_v[l])
        if last:
            x = x + g2 * _peer(hx2.reshape(B * L, D), *peer_args).reshape(B, L, D)
        else:
            ctx = ctx + g1c * yc
            hc2 = _rmsnorm(ctx, norm_ffn[l]) * (1 + sc2c) + sh2c
            f = _peer(jnp.concatenate([hc2, hx2], axis=1).reshape(B * (C + L), D), *peer_args)
            f = f.reshape(B, C + L, D)
            ctx = ctx + g2c * f[:, :C]
            x = x + g2 * f[:, C:]
    return _rmsnorm(x, final_norm)
```

```python
from contextlib import ExitStack
import numpy as np
import concourse.bass as bass
import concourse.mybir as mybir
from concourse.bass_utils import run_bass_kernel_spmd

F32 = mybir.dt.float32
BF16 = mybir.dt.bfloat16
U32 = mybir.dt.uint32
AF = mybir.ActivationFunctionType
ALU = mybir.AluOpType
AX = mybir.AxisListType

N_DSEM = 24
N_HW_DSEM = 14
EPS = 1e-6
NEG = -1e30
TS = 2304
NTL = 18
NFC = 24
NFS = 20
NTM = 1408


class Sched:
    ENGS = ("pe", "dve", "act", "pool", "sp")

    def __init__(self, nc, same_engine_sync=True):
        self.nc = nc
        self.q = {e: [] for e in self.ENGS}
        self.n = {e: 0 for e in self.ENGS}
        self.waited = {}
        self.last_w = {}
        self.readers = {}
        self.dma_i = 0
        self.dma_sw = 0
        self.dsem_uses = [0] * N_DSEM
        self.same = same_engine_sync
        self.final_tokens = []
        self.bar = []

    def _deps(self, reads, writes):
        deps = []
        for r in reads:
            t = self.last_w.get(r)
            if t is not None:
                deps.append(t)
        for w in writes:
            t = self.last_w.get(w)
            if t is not None:
                deps.append(t)
            deps.extend(self.readers.get(w, ()))
        deps.extend(self.bar)
        return deps

    def _waits_for(self, eng, deps):
        need = {}
        for t in deps:
            if t[0] == "eng":
                _, e2, c = t
                if e2 == eng and (not self.same or eng == "pe"):
                    continue
                key = ("eng", e2)
            else:
                _, k, c = t
                key = ("dma", k)
            if self.waited.get((eng, key), 0) >= c:
                continue
            if need.get(key, 0) < c:
                need[key] = c
        for key, c in need.items():
            self.waited[(eng, key)] = c
        return list(need.items())

    def _commit(self, tok, reads, writes):
        for w in writes:
            self.last_w[w] = tok
            self.readers[w] = []
        for r in reads:
            if r in writes:
                continue
            lst = self.readers.setdefault(r, [])
            lst.append(tok)
            if len(lst) > 48:
                best = {}
                for t in lst:
                    k = (t[0], t[1])
                    if k not in best or best[k][2] < t[2]:
                        best[k] = t
                self.readers[r] = list(best.values())

    def barrier(self):
        self.bar = [("eng", e, self.n[e]) for e in self.ENGS if self.n[e]]
        self.bar += [("dma", k_, 16 * self.dsem_uses[k_]) for k_ in range(N_DSEM) if self.dsem_uses[k_]]

    def op(self, eng, fn, reads=(), writes=()):
        reads, writes = tuple(reads), tuple(writes)
        waits = self._waits_for(eng, self._deps(reads, writes))
        self.n[eng] += 1
        tok = ("eng", eng, self.n[eng])
        self.q[eng].append((waits, fn, ("eng", eng)))
        self._commit(tok, reads, writes)
        return tok

    def dma(self, eng, fn, reads=(), writes=()):
        reads, writes = tuple(reads), tuple(writes)
        if eng == "pool":
            k = N_HW_DSEM + self.dma_sw % (N_DSEM - N_HW_DSEM)
            self.dma_sw += 1
        else:
            k = self.dma_i % N_HW_DSEM
            self.dma_i += 1
        prev = self.dsem_uses[k] * 16
        self.dsem_uses[k] += 1
        deps = self._deps(reads, writes)
        if prev > 0:
            deps.append(("dma", k, prev))
        waits = self._waits_for(eng, deps)
        tok = ("dma", k, prev + 16)
        self.q[eng].append((waits, fn, ("dma", k)))
        self._commit(tok, reads, writes)
        return tok

    def finish(self, tokens):
        self.final_tokens = list(tokens)

    def emit(self, stack):
        nc = self.nc
        esem = {e: stack.enter_context(nc.semaphore("s_" + e)) for e in self.ENGS}
        dsem = [stack.enter_context(nc.semaphore("d_%d" % i)) for i in range(N_DSEM)]
        with nc.Block() as b0:
            @b0.sync
            def _(h):
                for sm in list(esem.values()) + dsem:
                    h.sem_clear(sm)
        block = stack.enter_context(nc.Block())

        def semof(key):
            return esem[key[1]] if key[0] == "eng" else dsem[key[1]]

        def run(eng_name, h):
            for waits, fn, inc in self.q[eng_name]:
                for key, c in waits:
                    h.wait_ge(semof(key), c)
                ins = fn(h)
                if inc[0] == "eng":
                    ins.then_inc(esem[inc[1]], 1)
                else:
                    ins.then_inc(dsem[inc[1]], 16)
            if eng_name == "sp":
                for k_ in range(N_DSEM):
                    if self.dsem_uses[k_]:
                        h.wait_ge(dsem[k_], 16 * self.dsem_uses[k_])
                for e_ in self.ENGS:
                    if self.n[e_]:
                        h.wait_ge(esem[e_], self.n[e_])

        @block.tensor
        def _(h):
            run("pe", h)

        @block.vector
        def _(h):
            run("dve", h)

        @block.scalar
        def _(h):
            run("act", h)

        @block.gpsimd
        def _(h):
            run("pool", h)

        @block.sync
        def _(h):
            run("sp", h)


class Arena:
    def __init__(self, nc, stack, name, kbytes):
        self.cols = kbytes * 256
        self.t = stack.enter_context(nc.sbuf_tensor(name, [128, self.cols], F32))
        self.off = 0
        self.marks = []
        self.peak = 0
        self.on_release = None

    def alloc(self, n, dtype=F32):
        bpe = 2 if dtype == BF16 else 4
        words = (n * bpe + 3) // 4
        words = (words + 7) // 8 * 8
        assert self.off + words <= self.cols, ("SBUF arena overflow", self.off, words, self.cols)
        ap = self.t[:, self.off:self.off + words]
        self.off += words
        self.peak = max(self.peak, self.off)
        if dtype != F32:
            ap = ap.bitcast(dtype)
        return ap[:, 0:n]

    def mark(self):
        self.marks.append(self.off)

    def release(self):
        self.off = self.marks.pop()
        if self.on_release is not None:
            self.on_release()


class Prog:
    def __init__(self, depth=2, phases=None, dbg=(), ntile_peer=None):
        self.depth = depth
        self.phases = phases
        self.dbg = set(dbg)
        self.ntile_peer = ntile_peer
        self.nc = bass.Bass("TRN2", target_bir_lowering=False)
        self.uid = 0

    def want(self, l, p):
        if self.phases is None:
            return True
        if isinstance(p, str):
            sub = [q for q in self.phases if q[0] == l and isinstance(q[1], str)]
            return (l, p) in self.phases if sub else (l, 2) in self.phases
        return (l, p) in self.phases

    def u(self, base):
        self.uid += 1
        return "%s_%d" % (base, self.uid)

    def din(self, name, shape, dt=F32):
        return self.nc.dram_tensor(name, list(shape), dt, kind="ExternalInput").ap()

    def dscr(self, name, shape, dt=F32):
        kind = "ExternalOutput" if name in self.dbg else "Internal"
        return self.nc.dram_tensor(name, list(shape), dt, kind=kind).ap()

    def dump(self, name, ap, shape, reads, dt=F32):
        if name not in self.dbg:
            return
        d = self.nc.dram_tensor(name, list(shape), dt, kind="ExternalOutput").ap()
        self.ld(d, ap, reads, [self.u("dump")])

    def mm(self, out, lhsT, rhs, start, stop, r, w):
        return self.S.op("pe", lambda h: h.matmul(out, lhsT=lhsT, rhs=rhs, start=start, stop=stop), r, w)

    def tr(self, out, in_, ident, r, w):
        return self.S.op("pe", lambda h: h.transpose(out=out, in_=in_, identity=ident), r, w)

    def act(self, out, in_, func, r, w, bias=None, scale=None, accum=None, eng="act"):
        kw = {}
        if bias is not None:
            kw["bias"] = bias
        if scale is not None:
            kw["scale"] = scale
        if accum is not None:
            kw["accum_out"] = accum
        return self.S.op("act", lambda h: h.activation(out=out, in_=in_, func=func, **kw), r, w)

    def tt(self, out, in0, in1, op, r, w, eng="dve"):
        return self.S.op(eng, lambda h: h.tensor_tensor(out=out, in0=in0, in1=in1, op=op), r, w)

    def ts(self, out, in0, s1, s2, op0, op1, r, w, eng="dve"):
        if op1 is None:
            return self.S.op(eng, lambda h: h.tensor_scalar(out=out, in0=in0, scalar1=s1, scalar2=None, op0=op0), r, w)
        return self.S.op(eng, lambda h: h.tensor_scalar(out=out, in0=in0, scalar1=s1, scalar2=s2, op0=op0, op1=op1), r, w)

    def stt(self, out, in0, scalar, in1, op0, op1, r, w):
        return self.S.op("dve", lambda h: h.scalar_tensor_tensor(out=out, in0=in0, scalar=scalar, in1=in1, op0=op0, op1=op1), r, w)

    def cp(self, out, in_, r, w, eng="dve"):
        if eng == "act":
            return self.S.op("act", lambda h: h.activation(out=out, in_=in_, func=AF.Copy), r, w)
        return self.S.op(eng, lambda h: h.tensor_copy(out=out, in_=in_), r, w)

    def ld(self, out, in_, r, w, q="sp"):
        return self.S.dma(q, lambda h: h.dma_start(out=out, in_=in_), r, w)

    def memset(self, ap, val, w, eng="pool"):
        return self.S.op(eng, lambda h: h.memset(ap, val), (), w)

    def build(self):
        nc = self.nc
        D = self.depth
        I = {}
        I["xin"] = self.din("xin", [2, TS, 1024])
        I["cT"] = self.din("cT", [128, 24])
        I["w_mod"] = self.din("w_mod", [D, 1024, 6144])
        I["b_mod"] = self.din("b_mod", [D, 6144])
        I["norm_mix"] = self.din("norm_mix", [D, 1024])
        I["norm_ffn"] = self.din("norm_ffn", [D, 1024])
        I["WF"] = self.din("WF", [D, 1024, NFC * 128])
        I["WT"] = self.din("WT", [D, 1024, NTM])
        I["ropec"] = self.din("ropec", [128, 2048])
        I["ropes"] = self.din("ropes", [128, 2048])
        I["convT"] = self.din("convT", [D, 128, 6])
        I["sink"] = self.din("sink", [D, 4])
        I["BM"] = self.din("BM", [D, 128, 20 * 512])
        I["band"] = self.din("band", [128, 384])
        I["lbl"] = self.din("lbl", [2, 256])
        I["lblT"] = self.din("lblT", [128, 4])
        I["hconst"] = self.din("hconst", [128, 9 * 128])
        if self.phases is None or any(p[1] == 3 for p in self.phases):
            I["gnorm"] = self.din("gnorm", [D, 1024])
            I["w_out"] = self.din("w_out", [D, 1024, 1024])
            I["wq"] = self.din("wq", [D, 1024, 2048])
            I["kT"] = self.din("kT", [D, 128, 256])
            for l_ in range(D):
                I["uv%d" % l_] = self.din("uv%d" % l_, [16384, 2048])
            I["fnorm"] = self.din("fnorm", [1, 1024])
            I["iota16"] = self.din("iota16", [128, 16])
        self.I = I
        self.out = nc.dram_tensor("out", [2, 2048, 1024], F32, kind="ExternalOutput").ap()
        self.modv = self.dscr("modv", [3, 6, 1024])
        self.PF = self.dscr("PF", [NFS * 128, 2, TS], BF16)
        self.PT = self.dscr("PT", [2, TS, NTM], BF16)
        self.ymix = self.dscr("ymix", [2, TS, 1024])
        self.xres = self.dscr("xres", [2, TS, 1024])
        self.uvb = [self.dscr("uvb%d" % l_, [16384, 2048], BF16) for l_ in range(D)]
        self.dbgq = self.dscr("dbgq", [128, 128 * 4]) if "dbgq" in self.dbg else None

        with ExitStack() as st:
            self.ar = Arena(nc, st, "arena", 190)
            self.ps = st.enter_context(nc.psum_tensor("ps", [128, 4096], F32))
            self.S = Sched(nc)
            self.ar.on_release = self.S.barrier
            self.final = []
            self.setup_consts()
            if self.phases is None or any(p[1] == 3 for p in self.phases):
                self.phase_tables()
            for l in range(D):
                last = l == D - 1
                if self.want(l, 0):
                    self.phase_mod(l)
                if self.want(l, 1):
                    self.phase_proj(l)
                if self.want(l, 2):
                    self.phase_mix(l, last)
                if self.want(l, 3):
                    self.phase_ffn(l, last)
            self.S.finish(self.final)
            self.S.emit(st)
        return nc

    def bank(self, k):
        return self.ps[:, k * 512:(k + 1) * 512]

    def setup_consts(self):
        ar = self.ar
        self.identf = ar.alloc(128)
        self.identb = ar.alloc(128, BF16)
        self.memset(self.identf, 0.0, ["identf"])
        self.S.op("pool", lambda h: h.affine_select(out=self.identf, in_=self.identf, pattern=[[-1, 128]],
                                                     compare_op=ALU.not_equal, fill=1.0, base=0, channel_multiplier=1),
                  ["identf"], ["identf"])
        self.cp(self.identb, self.identf, ["identf"], ["identb"])
        self.junk = ar.alloc(1024, BF16)
        self.junkd = ar.alloc(1024, BF16)

    def rstd(self, out, ss, inv_n, tmp, r, w):
        self.ts(tmp, ss, inv_n, EPS, ALU.mult, ALU.add, r, [w + "_t"])
        self.act(tmp, tmp, AF.Sqrt, [w + "_t"], [w + "_t"])
        self.S.op("dve", lambda h: h.reciprocal(out=out, in_=tmp), [w + "_t"], [w])

    def phase_mod(self, l):
        ar, I = self.ar, self.I
        ar.mark()
        cT = ar.alloc(24)
        scT = ar.alloc(24)
        msb = ar.alloc(6144)
        bmb = ar.alloc(6144)
        nmx = ar.alloc(1024)
        nff = ar.alloc(1024)
        wch = [ar.alloc(8 * 512) for _ in range(2)]
        k = self.u("mod")
        self.ld(cT, I["cT"], [], [k + "cT"])
        self.act(scT, cT, AF.Silu, [k + "cT"], [k + "scT"])
        self.ld(bmb[0:3, :], I["b_mod"][l:l + 1, :].partition_broadcast(3), [], [k + "bmb"])
        self.ld(nmx[0:3, :], I["norm_mix"][l:l + 1, :].partition_broadcast(3), [], [k + "nmx"])
        self.ld(nff[0:3, :], I["norm_ffn"][l:l + 1, :].partition_broadcast(3), [], [k + "nff"])
        wv = I["w_mod"][l].rearrange("(k p) n -> p k n", p=128)
        for n in range(12):
            wb = wch[n % 2]
            wk = k + "w%d" % (n % 2)
            self.ld(wb.rearrange("p (k n) -> p k n", k=8), wv[:, :, n * 512:(n + 1) * 512], [], [wk])
            pb = self.bank(n % 2)
            pk = "ps%d" % (n % 2)
            for kk in range(8):
                self.mm(pb[0:3, :], scT[:, kk * 3:(kk + 1) * 3], wb[:, kk * 512:(kk + 1) * 512],
                        kk == 0, kk == 7, [k + "scT", wk], [pk])
            self.tt(msb[0:3, n * 512:(n + 1) * 512], pb[0:3, :], bmb[0:3, n * 512:(n + 1) * 512], ALU.add,
                    [pk, k + "bmb"], [k + "msb"])
        self.dump("d_scT", scT, [128, 24], [k + "scT"])
        self.dump("d_msb", msb[0:3, :], [3, 6144], [k + "msb"])
        self.stt(msb[0:3, 1024:2048], msb[0:3, 1024:2048], 1.0, nmx[0:3, :], ALU.add, ALU.mult,
                 [k + "msb", k + "nmx"], [k + "msb"])
        self.stt(msb[0:3, 4096:5120], msb[0:3, 4096:5120], 1.0, nff[0:3, :], ALU.add, ALU.mult,
                 [k + "msb", k + "nff"], [k + "msb"])
        order = [1, 0, 2, 4, 3, 5]
        for slot, j in enumerate(order):
            self.ld(self.modv[:, slot, :], msb[0:3, j * 1024:(j + 1) * 1024], [k + "msb"], ["modv"])
        ar.release()

    def load_bc(self, dst, slot, r, key):
        self.ld(dst, self.modv[r, slot:slot + 1, :].partition_broadcast(128), ["modv"], [key])

    def phase_proj(self, l):
        ar, I = self.ar, self.I
        ar.mark()
        k = self.u("pj")
        wF = ar.alloc(8 * NFC * 128, BF16)
        wT = ar.alloc(8 * NTM, BF16)
        wFv = wF.rearrange("p (k n) -> p k n", k=8)
        wTv = wT.rearrange("p (k n) -> p k n", k=8)
        srcF = I["WF"][l].rearrange("(k p) n -> p k n", p=128)
        srcT = I["WT"][l].rearrange("(k p) n -> p k n", p=128)
        for c in range(0, NFC * 128, 512):
            self.ld(wFv[:, :, c:c + 512], srcF[:, :, c:c + 512], [], [k + "wF"], q="pool")
        for c in range(0, NTM, 352):
            self.ld(wTv[:, :, c:c + 352], srcT[:, :, c:c + 352], [], [k + "wT"], q="pool")
        cosT = ar.alloc(2048)
        sinT = ar.alloc(2048)
        self.ld(cosT, I["ropec"], [], [k + "cos"])
        self.ld(sinT, I["ropes"], [], [k + "sin"])
        A1 = ar.alloc(1024)
        B1 = ar.alloc(1024)
        xt = [ar.alloc(1024) for _ in range(2)]
        tmpf = ar.alloc(1024)
        hx = ar.alloc(1024, BF16)
        hxT = ar.alloc(1024, BF16)
        pfs = [ar.alloc(NFS * 128, BF16) for _ in range(2)]
        pts = [ar.alloc(NTM, BF16) for _ in range(2)]
        ss = ar.alloc(1)
        rs = ar.alloc(1)
        tm1 = ar.alloc(1)
        rt1 = ar.alloc(128)
        rt2 = ar.alloc(128)
        xsrc = I["xin"] if l == 0 else self.xres
        tiles = [(b, i) for b in range(2) for i in range(NTL)]

        def xkey(b, i):
            return [] if l == 0 else [("xres", b, i)]

        def loads(n):
            b, i = tiles[n]
            self.ld(xt[n % 2], xsrc[b, i * 128:(i + 1) * 128, :], xkey(b, i), [k + "xt%d" % (n % 2)])

        loads(0)
        cur_r = None
        pfv_d = self.PF.rearrange("(c p) b s -> p c b s", p=128)
        for n, (b, i) in enumerate(tiles):
            if n + 1 < len(tiles):
                loads(n + 1)
            r = 2 if i < 2 else b
            if r != cur_r:
                self.load_bc(A1, 0, r, k + "A1")
                self.load_bc(B1, 1, r, k + "B1")
                cur_r = r
            x = xt[n % 2]
            xk = k + "xt%d" % (n % 2)
            self.act(self.junk, x, AF.Square, [xk], [k + "ss"], accum=ss)
            self.rstd(rs, ss, 1.0 / 1024, tm1, [k + "ss"], k + "rs")
            self.stt(tmpf, x, rs, A1, ALU.mult, ALU.mult, [xk, k + "rs", k + "A1"], [k + "tmpf"])
            self.tt(hx, tmpf, B1, ALU.add, [k + "tmpf", k + "B1"], [k + "hx"])
            pT = self.bank(0).bitcast(BF16)
            for c in range(8):
                self.tr(pT[:, c * 128:(c + 1) * 128], hx[:, c * 128:(c + 1) * 128], self.identb,
                        [k + "hx", "identb"], ["ps0"])
            self.cp(hxT, pT, ["ps0"], [k + "hxT"], eng="act")
            hxTv = hxT.rearrange("p (c t) -> p c t", c=8)
            pf = pfs[n % 2]
            pfk = k + "pf%d" % (n % 2)
            pfv = pf.rearrange("p (c t) -> p c t", c=NFS)
            latent = i >= 2
            t0 = i * 128 - 256
            for g in range(NFC // 4):
                bk = 1 + g % 3
                pk = "ps%d" % bk
                pb = self.bank(bk)
                for j in range(4):
                    fc = g * 4 + j
                    for kk in range(8):
                        self.mm(pb[:, j * 128:(j + 1) * 128], wFv[:, kk, fc * 128:(fc + 1) * 128], hxTv[:, kk, :],
                                kk == 0, kk == 7, [k + "wF", k + "hxT"], [pk])
                if g < 2:
                    for j in range(2):
                        dst = pfv[:, 2 * g + j, :]
                        if latent:
                            self.tt(rt1, pb[:, j * 128:(j + 1) * 128], cosT[:, t0:t0 + 128], ALU.mult,
                                    [pk, k + "cos"], [k + "rt1"])
                            self.tt(rt2, pb[:, (2 + j) * 128:(3 + j) * 128], sinT[:, t0:t0 + 128], ALU.mult,
                                    [pk, k + "sin"], [k + "rt2"])
                            self.tt(dst, rt1, rt2, ALU.add, [k + "rt1", k + "rt2"], [pfk])
                        else:
                            self.cp(dst, pb[:, j * 128:(j + 1) * 128], [pk], [pfk])
                else:
                    dst = pf[:, (4 + (g - 2) * 4) * 128:(8 + (g - 2) * 4) * 128]
                    self.cp(dst, pb, [pk], [pfk], eng=("act" if g % 2 else "dve"))
            self.ld(pfv_d[:, :, b, i * 128:(i + 1) * 128], pfv, [pfk], [("PF", b, i)])
            pt = pts[n % 2]
            ptk = k + "pt%d" % (n % 2)
            for g, (c0, c1) in enumerate([(0, 512), (512, 1024), (1024, NTM)]):
                bk = 4 + g
                pk = "ps%d" % bk
                pb = self.bank(bk)
                for kk in range(8):
                    self.mm(pb[:, 0:c1 - c0], hxTv[:, kk, :], wTv[:, kk, c0:c1], kk == 0, kk == 7,
                            [k + "wT", k + "hxT"], [pk])
                self.cp(pt[:, c0:c1], pb[:, 0:c1 - c0], [pk], [ptk], eng=("act" if g % 2 else "dve"))
            self.ld(self.PT[b, i * 128:(i + 1) * 128, :], pt, [ptk], [("PT", b, i)])
        ar.release()

    def attn_unit(self, k, nq_parts, score_mms, nloc, bias_ap, bias_key, nctx, sink_col, pv_list, out_ap, out_key, W, sink_key=None):
        S_sb, P_sb, PT_sb, m, negm, rsum, es, rinv = W["S"], W["P"], W["PT"], W["m"], W["negm"], W["rsum"], W["es"], W["rinv"]
        psA, psB, psT, psO = self.bank(0), self.bank(1), self.bank(2).bitcast(BF16), self.bank(3)
        for (o, lt, rh, rd, pk) in score_mms:
            self.mm(o, lt, rh, True, True, rd, [pk])
        ntot = nloc + nctx
        if nloc:
            self.stt(S_sb[:, 0:nloc], psA[:, 0:nloc], 0.125, bias_ap, ALU.mult, ALU.add, ["ps0", bias_key], [k + "S"])
        self.act(S_sb[:, nloc:ntot], psB[:, 0:nctx], AF.Copy, ["ps1"], [k + "S"], scale=0.125)
        self.S.op("dve", lambda h: h.reduce_max(out=m, in_=S_sb[:, 0:ntot], axis=AX.X), [k + "S"], [k + "m"])
        if sink_col is not None:
            self.tt(m, m, sink_col, ALU.max, [k + "m", sink_key], [k + "m"])
        self.ts(negm, m, -1.0, None, ALU.mult, None, [k + "m"], [k + "negm"])
        self.act(P_sb[:, 0:ntot], S_sb[:, 0:ntot], AF.Exp, [k + "S", k + "negm"], [k + "P", k + "rsum"], bias=negm, accum=rsum)
        if sink_col is not None:
            self.act(es, sink_col, AF.Exp, [sink_key, k + "negm"], [k + "es"], bias=negm)
            self.tt(rsum, rsum, es, ALU.add, [k + "rsum", k + "es"], [k + "rsum"])
        self.S.op("dve", lambda h: h.reciprocal(out=rinv, in_=rsum), [k + "rsum"], [k + "rinv"])
        nch = ntot // 128
        for c in range(nch):
            self.tr(psT[:, c * 128:(c + 1) * 128], P_sb[:, c * 128:(c + 1) * 128], self.identb, [k + "P", "identb"], ["ps2"])
        self.cp(PT_sb[:, 0:ntot], psT[:, 0:ntot], ["ps2"], [k + "PT"], eng="act")
        for (p0, p1, items) in pv_list:
            for ii, (c, rhs, rd) in enumerate(items):
                self.mm(psO[p0:p1, 0:64], PT_sb[:, c * 128 + p0:c * 128 + p1], rhs, ii == 0, ii == len(items) - 1,
                        [k + "PT"] + rd, ["ps3"])
        self.ts(out_ap, psO[:, 0:64], rinv, None, ALU.mult, None, ["ps3", k + "rinv"], [out_key])

    def phase_mix(self, l, last):
        ar, I = self.ar, self.I
        ar.mark()
        k = self.u("mx")
        band = ar.alloc(384)
        self.ld(band, I["band"], [], [k + "band"])
        sinkb = ar.alloc(4)
        self.ld(sinkb, I["sink"][l:l + 1, :].partition_broadcast(128), [], [k + "sink"])
        convT = ar.alloc(6)
        self.ld(convT, I["convT"][l], [], [k + "convT"])
        hc = ar.alloc(9 * 128)
        self.ld(hc, I["hconst"], [], [k + "hc"])
        lbm_bc = ar.alloc(256)
        oml_bc = ar.alloc(256)
        lbT = ar.alloc(4)
        lbm_pp = ar.alloc(2)
        oml_pp = ar.alloc(2)
        if l == 0:
            self.memset(lbm_bc, 1e-20, [k + "lbm"])
            self.memset(oml_bc, 1.0, [k + "oml"])
            self.memset(lbm_pp, 1e-20, [k + "lbpp"])
            self.memset(oml_pp, 1.0, [k + "lbpp"])
        else:
            l0 = ar.alloc(256)
            self.ld(l0, I["lbl"][0:1, :].partition_broadcast(128), [], [k + "l0"])
            self.ld(lbm_bc, I["lbl"][1:2, :].partition_broadcast(128), [], [k + "lbm"])
            self.tt(lbm_bc, lbm_bc, l0, ALU.subtract, [k + "l0", k + "lbm"], [k + "lbm"])
            self.act(oml_bc, lbm_bc, AF.Sigmoid, [k + "lbm"], [k + "oml"], scale=-1.0)
            self.act(lbm_bc, lbm_bc, AF.Sigmoid, [k + "lbm"], [k + "lbm"])
            self.ts(lbm_bc, lbm_bc, 1e-20, None, ALU.max, None, [k + "lbm"], [k + "lbm"])
            self.ld(lbT, I["lblT"], [], [k + "lbT"])
            self.tt(lbm_pp, lbT[:, 2:4], lbT[:, 0:2], ALU.subtract, [k + "lbT"], [k + "lbpp"])
            self.act(oml_pp, lbm_pp, AF.Sigmoid, [k + "lbpp"], [k + "lbpp"], scale=-1.0)
            self.act(lbm_pp, lbm_pp, AF.Sigmoid, [k + "lbpp"], [k + "lbpp"])
            self.ts(lbm_pp, lbm_pp, 1e-20, None, ALU.max, None, [k + "lbpp"], [k + "lbpp"])
        PFv = self.PF.rearrange("(c p) b s -> p c b s", p=128)
        for b in range(2):
            pfk = [("PF", b, i) for i in range(NTL)]
            ptk = [("PT", b, i) for i in range(NTL)]
            PTv = self.PT[b].rearrange("(i p) c -> p i c", p=128)
            if self.want(l, "A") or self.want(l, "B"):
                self.mix_attn(l, last, b, k, band, sinkb, PFv, PTv, pfk, ptk)
            if self.want(l, "C"):
                self.mix_conv(l, last, b, k, convT, PFv, pfk)
            if self.want(l, "D"):
                self.mix_hgrn(l, last, b, k, hc, lbm_bc, oml_bc, lbm_pp, oml_pp, PFv, PTv, pfk, ptk)
        ar.release()

    def mix_attn(self, l, last, b, k0, band, sinkb, PFv, PTv, pfk, ptk):
        ar, I = self.ar, self.I
        ar.mark()
        k = self.u(k0 + "at")
        BM = ar.alloc(20 * 512)
        self.ld(BM, I["BM"][l], [], [k + "BM"])
        BMv = BM.rearrange("p (c n) -> p c n", c=20)
        q = {}
        for nm, c0 in (("qA", 0), ("kA", 2), ("qB", 4), ("kB", 6)):
            t = ar.alloc(2 * TS, BF16)
            tv = t.rearrange("p (c s) -> p c s", c=2)
            self.ld(tv, PFv[:, c0:c0 + 2, b, :], pfk, [k + nm])
            q[nm] = tv
        vA = ar.alloc(NTL * 128, BF16).rearrange("p (i c) -> p i c", i=NTL)
        vB = ar.alloc(NTL * 256, BF16).rearrange("p (i c) -> p i c", i=NTL)
        vBs = ar.alloc(15 * 256, BF16).rearrange("p (i c) -> p i c", i=15)
        self.ld(vA, PTv[:, :, 0:128], ptk, [k + "vA"])
        self.ld(vB, PTv[:, :, 128:384], ptk, [k + "vB"])
        PTs = self.PT[b, 320:320 + 15 * 128, :].rearrange("(i p) c -> p i c", p=128)
        self.ld(vBs, PTs[:, :, 128:384], ptk, [k + "vBs"])
        W = dict(S=ar.alloc(768), P=ar.alloc(768, BF16), PT=ar.alloc(768, BF16), m=ar.alloc(1), negm=ar.alloc(1),
                 rsum=ar.alloc(1), es=ar.alloc(1), rinv=ar.alloc(1))
        yo = [ar.alloc(256) for _ in range(2)]
        psA, psB = self.bank(0), self.bank(1)
        cnt = 0
        if self.want(l, "A"):
            units = [("lat", n) for n in range(16)]
            if not last:
                units += [("ctx", n) for n in range(2)]
            for kind, n in units:
                y = yo[cnt % 2]
                yk = k + "yo%d" % (cnt % 2)
                cnt += 1
                for h in range(4):
                    fc, pb = h // 2, (h % 2) * 64
                    if kind == "lat":
                        t_lo, t_hi = max(0, 128 * n - 128), min(2048, 128 * n + 256)
                        nk = t_hi - t_lo
                        moff = t_lo - (128 * n - 128)
                        qcols = slice(256 + 128 * n, 256 + 128 * n + 128)
                    else:
                        t_lo = nk = moff = 0
                        qcols = slice(128 * n, 128 * n + 128)
                    lt = q["qA"][pb:pb + 64, fc, qcols]
                    sm = []
                    if nk:
                        sm.append((psA[:, 0:nk], lt, q["kA"][pb:pb + 64, fc, 256 + t_lo:256 + t_lo + nk], [k + "qA", k + "kA"], "ps0"))
                    sm.append((psB[:, 0:256], lt, q["kA"][pb:pb + 64, fc, 0:256], [k + "qA", k + "kA"], "ps1"))
                    items = []
                    for c in range(nk // 128):
                        items.append((c, vA[:, 2 + t_lo // 128 + c, fc * 64:(fc + 1) * 64], [k + "vA"]))
                    for c in range(2):
                        items.append((nk // 128 + c, vA[:, c, fc * 64:(fc + 1) * 64], [k + "vA"]))
                    self.attn_unit(k, 1, sm, nk, band[:, moff:moff + nk] if nk else None, k0 + "band",
                                   256, sinkb[:, h:h + 1], [(0, 128, items)], y[:, h * 64:(h + 1) * 64], yk, W, sink_key=k0 + "sink")
                s0 = 256 + 128 * n if kind == "lat" else 128 * n
                self.ld(self.ymix[b, s0:s0 + 128, 0:256], y, [yk], [("ymix", b, s0 // 128, 0)])
        if self.want(l, "B"):
            units = [("lat", rp) for rp in range(16)]
            if not last:
                units += [("ctx", n) for n in range(2)]
            for kind, rp in units:
                y = yo[cnt % 2]
                yk = k + "yo%d" % (cnt % 2)
                cnt += 1
                for h in range(4):
                    fc, pb = h // 2, (h % 2) * 64
                    sm = []
                    pv = []
                    if kind == "lat":
                        cls = 0 if rp == 0 else 1 if rp == 1 else 3 if rp == 14 else 4 if rp == 15 else 2
                        for hh in range(2):
                            r = 2 * rp + hh
                            r0 = min(max(r - 4, 0), 24)
                            lt = q["qB"][pb:pb + 64, fc, 256 + 64 * r:256 + 64 * r + 64]
                            sm.append((psA[hh * 64:(hh + 1) * 64, 0:512], lt, q["kB"][pb:pb + 64, fc, 256 + 64 * r0:256 + 64 * r0 + 512],
                                       [k + "qB", k + "kB"], "ps0"))
                            sm.append((psB[hh * 64:(hh + 1) * 64, 0:256], lt, q["kB"][pb:pb + 64, fc, 0:256], [k + "qB", k + "kB"], "ps1"))
                            items = []
                            for c in range(4):
                                if r0 % 2 == 0:
                                    items.append((c, vB[:, 2 + r0 // 2 + c, h * 64:(h + 1) * 64], [k + "vB"]))
                                else:
                                    items.append((c, vBs[:, (r0 - 1) // 2 + c, h * 64:(h + 1) * 64], [k + "vBs"]))
                            for c in range(2):
                                items.append((4 + c, vB[:, c, h * 64:(h + 1) * 64], [k + "vB"]))
                            pv.append((hh * 64, hh * 64 + 64, items))
                        self.attn_unit(k, 2, sm, 512, BMv[:, cls * 4 + h, :], k + "BM", 256, None, pv,
                                       y[:, h * 64:(h + 1) * 64], yk, W)
                    else:
                        lt = q["qB"][pb:pb + 64, fc, 128 * rp:128 * rp + 128]
                        sm.append((psB[:, 0:256], lt, q["kB"][pb:pb + 64, fc, 0:256], [k + "qB", k + "kB"], "ps1"))
                        items = [(c, vB[:, c, h * 64:(h + 1) * 64], [k + "vB"]) for c in range(2)]
                        self.attn_unit(k, 1, sm, 0, None, None, 256, None, [(0, 128, items)], y[:, h * 64:(h + 1) * 64], yk, W)
                s0 = 256 + 128 * rp if kind == "lat" else 128 * rp
                self.ld(self.ymix[b, s0:s0 + 128, 256:512], y, [yk], [("ymix", b, s0 // 128, 1)])
        ar.release()

    def mix_conv(self, l, last, b, k0, convT, PFv, pfk):
        ar = self.ar
        ar.mark()
        k = self.u(k0 + "cv")
        cin = ar.alloc(6 * TS, BF16).rearrange("p (c s) -> p c s", c=6)
        self.ld(cin, PFv[:, 14:20, b, :], pfk, [k + "cin"])
        u = ar.alloc(TS)
        acc = ar.alloc(TS)
        ycs = ar.alloc(NTL * 256).rearrange("p (i c) -> p i c", i=NTL)
        rngs = [(256, TS)] if last else [(0, 256), (256, TS)]
        for dt in range(2):
            self.tt(u, cin[:, 4 + dt, :], cin[:, 0 + dt, :], ALU.mult, [k + "cin"], [k + "u"])
            for (a, e) in rngs:
                w0, w1, w2 = (convT[:, dt * 3 + j:dt * 3 + j + 1] for j in range(3))
                self.ts(acc[:, a:e], u[:, a:e], w1, None, ALU.mult, None, [k + "u", k0 + "convT"], [k + "acc"])
                self.stt(acc[:, a + 1:e], u[:, a:e - 1], w0, acc[:, a + 1:e], ALU.mult, ALU.add, [k + "u", k + "acc"], [k + "acc"])
                self.stt(acc[:, a:e - 1], u[:, a + 1:e], w2, acc[:, a:e - 1], ALU.mult, ALU.add, [k + "u", k + "acc"], [k + "acc"])
            self.tt(acc, acc, cin[:, 2 + dt, :], ALU.mult, [k + "acc", k + "cin"], [k + "acc"])
            for i in range(0 if not last else 2, NTL):
                bk = 4 + i % 2
                pk = "ps%d" % bk
                self.tr(self.bank(bk)[:, 0:128], acc[:, i * 128:(i + 1) * 128], self.identf, [k + "acc", "identf"], [pk])
                self.cp(ycs[:, i, dt * 128:(dt + 1) * 128], self.bank(bk)[:, 0:128], [pk], [k + "ycs"], eng=("act" if i % 2 else "dve"))
        i0 = 0 if not last else 2
        dst = self.ymix[b].rearrange("(i p) c -> p i c", p=128)
        self.ld(dst[:, i0:NTL, 512:768], ycs[:, i0:NTL, :], [k + "ycs"], [("ymix", b, i, 2) for i in range(i0, NTL)])
        ar.release()

    def mix_hgrn(self, l, last, b, k0, hc, lbm_bc, oml_bc, lbm_pp, oml_pp, PFv, PTv, pfk, ptk):
        ar = self.ar
        ar.mark()
        k = self.u(k0 + "hg")
        hcv = hc.rearrange("p (c n) -> p c n", c=9)
        qz = ar.alloc(6 * TS, BF16).rearrange("p (c s) -> p c s", c=6)
        self.ld(qz, PFv[:, 8:14, b, :], pfk, [k + "qz"])
        od = ar.alloc(NTL * 256).rearrange("p (i c) -> p i c", i=NTL)
        Sst = ar.alloc(128)
        Sbf = ar.alloc(128, BF16)
        tok = [ar.alloc(1024, BF16) for _ in range(2)]
        sg = ar.alloc(256)
        lf = ar.alloc(256)
        kft = ar.alloc(256)
        kfT = ar.alloc(256)
        E13 = ar.alloc(512)
        E2T = ar.alloc(256)
        E4 = ar.alloc(256)
        dec = ar.alloc(4)
        qtil = ar.alloc(256, BF16)
        ktil = ar.alloc(256, BF16)
        qhat = ar.alloc(256, BF16)
        khat = ar.alloc(256, BF16)
        attTs = [ar.alloc(128, BF16) for _ in range(2)]
        gsl = ar.alloc(256)
        yd = [ar.alloc(256) for _ in range(2)]
        for d in range(2):
            order = list(range(NTL)) if d == 0 else [1, 0] + list(range(NTL - 1, 1, -1))
            cTri, cX, cE = hcv[:, 3 * d + 0, :], hcv[:, 3 * d + 1, :], hcv[:, 3 * d + 2, :]
            csel = hcv[:, 6, 0:2]
            attT = attTs[d]
            self.memset(attT, 0.0, [k + "attT0"])
            self.memset(Sst, 0.0, [k + "S"])
            self.memset(Sbf, 0.0, [k + "Sbf"])

            def loads(n):
                i = order[n]
                self.ld(tok[n % 2], PTv[:, i, 384:1408], [ptk[i]], [k + "tok%d" % (n % 2)])

            loads(0)
            for n, i in enumerate(order):
                if n + 1 < len(order):
                    loads(n + 1)
                tk = tok[n % 2]
                tkk = k + "tok%d" % (n % 2)
                zt = tk[:, d * 256:(d + 1) * 256]
                it = tk[:, 512:768]
                cols = slice(i * 128, (i + 1) * 128)
                self.act(sg, zt, AF.Sigmoid, [tkk], [k + "sg"])
                self.tt(sg, sg, oml_bc, ALU.mult, [k + "sg", k0 + "oml"], [k + "sg"])
                self.tt(sg, sg, lbm_bc, ALU.add, [k + "sg", k0 + "lbm"], [k + "sg"])
                self.act(lf, sg, AF.Ln, [k + "sg"], [k + "lf"])
                self.act(kft, zt, AF.Sigmoid, [tkk], [k + "kft"], scale=-1.0)
                self.tt(kft, kft, oml_bc, ALU.mult, [k + "kft", k0 + "oml"], [k + "kft"])
                for dt in range(2):
                    self.act(kfT[:, dt * 128:(dt + 1) * 128], qz[:, 2 + 2 * d + dt, cols], AF.Sigmoid, [k + "qz"], [k + "kfT"], scale=-1.0)
                    self.ts(kfT[:, dt * 128:(dt + 1) * 128], kfT[:, dt * 128:(dt + 1) * 128], oml_pp[:, dt:dt + 1], None, ALU.mult, None,
                            [k + "kfT", k0 + "lbpp"], [k + "kfT"])
                b0, b1 = self.bank(0), self.bank(1)
                for dt in range(2):
                    lt = lf[:, dt * 128:(dt + 1) * 128]
                    self.mm(b0[:, dt * 128:(dt + 1) * 128], lt, cTri, True, True, [k + "lf", k0 + "hc"], ["ps0"])
                    self.mm(b0[:, 256 + dt * 128:256 + (dt + 1) * 128], lt, cX, True, True, [k + "lf", k0 + "hc"], ["ps0"])
                    self.mm(b1[:, 256 + dt * 2:256 + dt * 2 + 2], lt, csel, True, True, [k + "lf", k0 + "hc"], ["ps1"])
                self.mm(b1[:, 0:256], cE, lf, True, True, [k + "lf", k0 + "hc"], ["ps1"])
                self.act(E13, b0, AF.Exp, ["ps0"], [k + "E13"])
                self.act(E2T, b0[:, 256:512], AF.Exp, ["ps0"], [k + "E2T"], scale=-1.0)
                self.act(E4, b1[:, 0:256], AF.Exp, ["ps1"], [k + "E4"])
                self.act(dec, b1[:, 256:260], AF.Exp, ["ps1"], [k + "dec"])
                qTv = qz[:, 0:2, cols]
                self.tt(qtil.rearrange("p (c t) -> p c t", c=2), qTv, E13[:, 256:512].rearrange("p (c t) -> p c t", c=2), ALU.mult,
                        [k + "qz", k + "E13"], [k + "qtil"])
                self.tt(qhat.rearrange("p (c t) -> p c t", c=2), qTv, E13[:, 0:256].rearrange("p (c t) -> p c t", c=2), ALU.mult,
                        [k + "qz", k + "E13"], [k + "qhat"])
                self.tt(ktil, kfT, E2T, ALU.mult, [k + "kfT", k + "E2T"], [k + "ktil"])
                self.tt(khat, kft, E4, ALU.mult, [k + "kft", k + "E4"], [k + "khat"])
                psO = self.bank(4)
                corder = (0, 1) if d == 0 else (1, 0)
                for dt in range(2):
                    for hp in range(2):
                        h = dt * 2 + hp
                        pb = hp * 64
                        bk = 2 + h % 2
                        pk = "ps%d" % bk
                        self.mm(self.bank(bk)[:, 0:128], ktil[pb:pb + 64, dt * 128:(dt + 1) * 128], qtil[pb:pb + 64, dt * 128:(dt + 1) * 128],
                                True, True, [k + "ktil", k + "qtil"], [pk])
                        self.S.op("dve", lambda h, bk=bk, attT=attT, cTri=cTri: h.copy_predicated(
                            out=attT, mask=cTri.bitcast(U32), data=self.bank(bk)[:, 0:128]), [pk, k0 + "hc", k + "attT0"], [k + "attT"])
                        osl = psO[:, h * 64:(h + 1) * 64]
                        self.mm(osl, attT, it[:, h * 64:(h + 1) * 64], True, False, [k + "attT", tkk], ["ps4"])
                        for ci, c in enumerate(corder):
                            self.mm(psO[c * 64:(c + 1) * 64, h * 64:(h + 1) * 64],
                                    qhat[pb:pb + 64, dt * 128 + c * 64:dt * 128 + c * 64 + 64],
                                    Sbf[pb:pb + 64, dt * 64:(dt + 1) * 64], False, True,
                                    [k + "qhat", k + "Sbf", k + "Sbf%d_%d" % (dt, hp)], ["ps4"])
                            if ci == 0:
                                self.state_update(k, d, dt, hp, c, khat, it, dec, Sst, Sbf, tkk)
                        self.state_update(k, d, dt, hp, corder[1], khat, it, dec, Sst, Sbf, tkk)
                if d == 0:
                    self.cp(od[:, i, :], psO[:, 0:256], ["ps4"], [k + "od%d" % i], eng="act")
                else:
                    self.tt(od[:, i, :], psO[:, 0:256], od[:, i, :], ALU.add, ["ps4", k + "od%d" % i], [k + "od%d" % i])
                    if not (last and i < 2):
                        y = yd[n % 2]
                        yk = k + "yd%d" % (n % 2)
                        self.act(gsl, tk[:, 768:1024], AF.Silu, [tkk], [k + "gsl"])
                        self.tt(y, od[:, i, :], gsl, ALU.mult, [k + "od%d" % i, k + "gsl"], [yk])
                        self.ld(self.ymix[b, i * 128:(i + 1) * 128, 768:1024], y, [yk], [("ymix", b, i, 3)])
        ar.release()

    def state_update(self, k, d, dt, hp, c, khat, it, dec, Sst, Sbf, tkk):
        h = dt * 2 + hp
        pb = hp * 64
        bk = 5 + (h % 2)
        pk = "ps%d" % bk
        psD = self.bank(bk)
        self.mm(psD[pb:pb + 64, 0:64], khat[c * 64:(c + 1) * 64, h * 64:(h + 1) * 64], it[c * 64:(c + 1) * 64, h * 64:(h + 1) * 64],
                True, True, [k + "khat", tkk], [pk])
        sk = k + "S%d_%d" % (dt, hp)
        self.stt(Sst[pb:pb + 64, dt * 64:(dt + 1) * 64], Sst[pb:pb + 64, dt * 64:(dt + 1) * 64], dec[pb:pb + 64, dt * 2 + c:dt * 2 + c + 1],
                 psD[pb:pb + 64, 0:64], ALU.mult, ALU.add, [k + "S", sk, k + "dec", pk], [sk])
        self.cp(Sbf[pb:pb + 64, dt * 64:(dt + 1) * 64], Sst[pb:pb + 64, dt * 64:(dt + 1) * 64], [sk, k + "S", k + "Sbf"], [k + "Sbf%d_%d" % (dt, hp)], eng="act")

    def phase_tables(self):
        ar, I = self.ar, self.I
        ar.mark()
        k = self.u("tb")
        RJ = 4
        stg = [ar.alloc(RJ * 2048, BF16) for _ in range(3)]
        n = 0
        for l in range(self.depth):
            if not any(self.want(l, 3) for _ in (0,)):
                continue
            src = I["uv%d" % l].rearrange("(p j) n -> p j n", p=128)
            dst = self.uvb[l].rearrange("(p j) n -> p j n", p=128)
            for j0 in range(0, 128, RJ):
                sb = stg[n % 3]
                sk = k + "stg%d" % (n % 3)
                n += 1
                self.ld(sb.rearrange("p (j n) -> p j n", j=RJ), src[:, j0:j0 + RJ, :], [], [sk], q="pool")
                self.ld(dst[:, j0:j0 + RJ, :], sb.rearrange("p (j n) -> p j n", j=RJ), [sk], [("uvb", l)])
        ar.release()

    def phase_ffn(self, l, last):
        ar, I = self.ar, self.I
        ar.mark()
        k = self.u("ff")
        wout = ar.alloc(8 * 1024, BF16).rearrange("p (k n) -> p k n", k=8)
        wq = ar.alloc(8 * 2048, BF16).rearrange("p (k n) -> p k n", k=8)
        kT = ar.alloc(256, BF16)
        src = I["w_out"][l].rearrange("(k p) n -> p k n", p=128)
        for c in range(0, 1024, 512):
            self.ld(wout[:, :, c:c + 512], src[:, :, c:c + 512], [], [k + "wout"], q="pool")
        src = I["wq"][l].rearrange("(k p) n -> p k n", p=128)
        for c in range(0, 2048, 512):
            self.ld(wq[:, :, c:c + 512], src[:, :, c:c + 512], [], [k + "wq"], q="pool")
        self.ld(kT, I["kT"][l], [], [k + "kT"], q="pool")
        gn = ar.alloc(1024)
        self.ld(gn, I["gnorm"][l:l + 1, :].partition_broadcast(128), [], [k + "gn"])
        iota = ar.alloc(16)
        self.ld(iota, I["iota16"], [], [k + "iota"])
        fn = None
        if last:
            fn = ar.alloc(1024)
            self.ld(fn, I["fnorm"].partition_broadcast(128), [], [k + "fn"])
        G1, A2, B2, G2 = (ar.alloc(1024) for _ in range(4))
        ym = [ar.alloc(1024)] * 2
        xt = [ar.alloc(1024)] * 2
        yn = ar.alloc(1024, BF16)
        ynT = ar.alloc(1024, BF16)
        xm = ar.alloc(1024)
        tmpf = ar.alloc(1024)
        hx2 = ar.alloc(1024)
        hx2b = yn
        hx2T = ynT
        qT = ar.alloc(16 * 128, BF16)
        sc = ar.alloc(16 * 128)
        work = ar.alloc(256)
        vals = ar.alloc(256)
        idxu = ar.alloc(256, U32)
        idxf = ar.alloc(256)
        cand = sc
        tops = ar.alloc(128)
        posu = ar.alloc(128, U32)
        pij = ar.alloc(256, U32)
        pijf = ar.alloc(256)
        oh = sc
        asel = ar.alloc(256)
        eidf = ar.alloc(128)
        eid = ar.alloc(128, U32)
        gate = ar.alloc(128)
        ntop = ar.alloc(8)
        zs = ar.alloc(8)
        sdot = ar.alloc(128)
        actv = ar.alloc(128)
        t1 = ar.alloc(128)
        t2 = ar.alloc(128)
        ssg = ar.alloc(4)
        rg = ar.alloc(4)
        tg = ar.alloc(4)
        ss = ar.alloc(1)
        rs = ar.alloc(1)
        tm1 = ar.alloc(1)
        NUV = 12
        UV = [ar.alloc(2048, BF16) for _ in range(NUV)]
        dg = [ar.alloc(128, BF16) for _ in range(8)]
        xo = [ar.alloc(1024)] * 2
        xsrc = I["xin"] if l == 0 else self.xres
        tiles = [(b, i) for b in range(2) for i in range(NTL) if not (last and i < 2)]
        if self.ntile_peer is not None:
            tiles = tiles[:self.ntile_peer]

        def loads(n):
            b, i = tiles[n]
            self.ld(ym[n % 2], self.ymix[b, i * 128:(i + 1) * 128, :], [("ymix", b, i, j) for j in range(4)], [k + "ym"])
            self.ld(xt[n % 2], xsrc[b, i * 128:(i + 1) * 128, :], [] if l == 0 else [("xres", b, i)], [k + "xt"])

        loads(0)
        cur_r = None
        gcount = 0
        for n, (b, i) in enumerate(tiles):
            r = 2 if i < 2 else b
            if r != cur_r:
                for dst, slot, nm in ((G1, 2, "G1"), (A2, 3, "A2"), (B2, 4, "B2"), (G2, 5, "G2")):
                    self.load_bc(dst, slot, r, k + nm)
                cur_r = r
            y, x = ym[n % 2], xt[n % 2]
            yk, xk = k + "ym", k + "xt"
            for g in range(4):
                self.act(self.junk[:, 0:256], y[:, g * 256:(g + 1) * 256], AF.Square, [yk], [k + "ssg"], accum=ssg[:, g:g + 1])
            self.rstd(rg, ssg, 1.0 / 256, tg, [k + "ssg"], k + "rg")
            for g in range(4):
                self.stt(yn[:, g * 256:(g + 1) * 256], y[:, g * 256:(g + 1) * 256], rg[:, g:g + 1], gn[:, g * 256:(g + 1) * 256],
                         ALU.mult, ALU.mult, [yk, k + "rg", k + "gn"], [k + "yn"])
            pT = self.bank(0).bitcast(BF16)
            for c in range(8):
                self.tr(pT[:, c * 128:(c + 1) * 128], yn[:, c * 128:(c + 1) * 128], self.identb, [k + "yn", "identb"], ["ps0"])
            self.cp(ynT, pT, ["ps0"], [k + "ynT"], eng="act")
            ynTv = ynT.rearrange("p (c t) -> p c t", c=8)
            for nn in range(2):
                pk = "ps%d" % (1 + nn)
                for kk in range(8):
                    self.mm(self.bank(1 + nn), ynTv[:, kk, :], wout[:, kk, nn * 512:(nn + 1) * 512], kk == 0, kk == 7,
                            [k + "ynT", k + "wout"], [pk])
            psY = self.ps[:, 512:1536]
            self.tt(tmpf, psY, G1, ALU.mult, ["ps1", "ps2", k + "G1"], [k + "tmpf"])
            self.tt(xm, tmpf, x, ALU.add, [k + "tmpf", xk], [k + "xm"])
            if n + 1 < len(tiles):
                loads(n + 1)
            self.act(self.junk, xm, AF.Square, [k + "xm"], [k + "ss"], accum=ss)
            self.rstd(rs, ss, 1.0 / 1024, tm1, [k + "ss"], k + "rs")
            self.stt(tmpf, xm, rs, A2, ALU.mult, ALU.mult, [k + "xm", k + "rs", k + "A2"], [k + "tmpf"])
            self.tt(hx2, tmpf, B2, ALU.add, [k + "tmpf", k + "B2"], [k + "hx2"])
            self.cp(hx2b, hx2, [k + "hx2"], [k + "yn"], eng="act")
            for c in range(8):
                self.tr(pT[:, c * 128:(c + 1) * 128], hx2b[:, c * 128:(c + 1) * 128], self.identb, [k + "yn", "identb"], ["ps0"])
            self.cp(hx2T, pT, ["ps0"], [k + "ynT"], eng="act")
            hx2Tv = hx2T.rearrange("p (c t) -> p c t", c=8)
            for g in range(4):
                bk = 3 + g % 2
                pk = "ps%d" % bk
                for j in range(4):
                    qc = g * 4 + j
                    for kk in range(8):
                        self.mm(self.bank(bk)[:, j * 128:(j + 1) * 128], wq[:, kk, qc * 128:(qc + 1) * 128], hx2Tv[:, kk, :],
                                kk == 0, kk == 7, [k + "wq", k + "ynT"], [pk])
                self.cp(qT[:, g * 512:(g + 1) * 512], self.bank(bk), [pk], [k + "qT"], eng=("act" if g % 2 else "dve"))
            for g in range(4):
                bk = 3 + g % 2
                pk = "ps%d" % bk
                for j in range(4):
                    qc = g * 4 + j
                    self.mm(self.bank(bk)[:, j * 128:(j + 1) * 128], qT[:, qc * 128:(qc + 1) * 128], kT[:, (qc % 2) * 128:(qc % 2 + 1) * 128],
                            True, True, [k + "qT", k + "kT"], [pk])
                self.cp(sc[:, g * 512:(g + 1) * 512], self.bank(bk), [pk], [k + "sc"], eng=("act" if g % 2 else "dve"))
            for qc in range(16):
                s_ = sc[:, qc * 128:(qc + 1) * 128]
                v_ = vals[:, qc * 16:(qc + 1) * 16]
                i_ = idxu[:, qc * 16:(qc + 1) * 16]
                self.S.op("dve", lambda h, s_=s_, v_=v_: h.max(out=v_[:, 0:8], in_=s_), [k + "sc"], [k + "vals"])
                self.S.op("dve", lambda h, s_=s_, v_=v_, i_=i_: h.max_index(out=i_[:, 0:8], in_max=v_[:, 0:8], in_values=s_), [k + "sc", k + "vals"], [k + "idxu"])
                self.S.op("dve", lambda h, s_=s_, v_=v_: h.match_replace(out=work[:, 0:128], in_to_replace=v_[:, 0:8], in_values=s_, imm_value=NEG),
                          [k + "sc", k + "vals"], [k + "work"])
                self.S.op("dve", lambda h, v_=v_: h.max(out=v_[:, 8:16], in_=work[:, 0:128]), [k + "work"], [k + "vals"])
                self.S.op("dve", lambda h, v_=v_, i_=i_: h.max_index(out=i_[:, 8:16], in_max=v_[:, 8:16], in_values=work[:, 0:128]),
                          [k + "work", k + "vals"], [k + "idxu"])
            self.cp(idxf, idxu, [k + "idxu"], [k + "idxf"])
            v4 = vals.rearrange("p (h f i) -> p h f i", h=8, f=2)
            candv = cand.rearrange("p (h i j) -> p h i j", h=8, i=16)
            for h_ in range(8):
                self.tt(candv[:, h_], v4[:, h_, 0, :].unsqueeze(2).broadcast_to([128, 16, 16]),
                        v4[:, h_, 1, :].unsqueeze(1).broadcast_to([128, 16, 16]), ALU.add, [k + "vals", k + "sc"], [k + "cand", k + "sc"])
            for h_ in range(8):
                c_ = cand[:, h_ * 256:(h_ + 1) * 256]
                t_ = tops[:, h_ * 16:(h_ + 1) * 16]
                p_ = posu[:, h_ * 16:(h_ + 1) * 16]
                self.S.op("dve", lambda h, c_=c_, t_=t_: h.max(out=t_[:, 0:8], in_=c_), [k + "cand"], [k + "tops"])
                self.S.op("dve", lambda h, c_=c_, t_=t_, p_=p_: h.max_index(out=p_[:, 0:8], in_max=t_[:, 0:8], in_values=c_), [k + "cand", k + "tops"], [k + "posu"])
                self.S.op("dve", lambda h, c_=c_, t_=t_: h.match_replace(out=work, in_to_replace=t_[:, 0:8], in_values=c_, imm_value=NEG),
                          [k + "cand", k + "tops"], [k + "work"])
                self.S.op("dve", lambda h, t_=t_: h.max(out=t_[:, 8:16], in_=work), [k + "work"], [k + "tops"])
                self.S.op("dve", lambda h, t_=t_, p_=p_: h.max_index(out=p_[:, 8:16], in_max=t_[:, 8:16], in_values=work), [k + "work", k + "tops"], [k + "posu"])
            self.S.op("dve", lambda h: h.tensor_single_scalar(out=pij[:, 0:128], in_=posu, scalar=4, op=ALU.logical_shift_right), [k + "posu"], [k + "pij"])
            self.S.op("dve", lambda h: h.tensor_single_scalar(out=pij[:, 128:256], in_=posu, scalar=15, op=ALU.bitwise_and), [k + "posu"], [k + "pij"])
            self.cp(pijf, pij, [k + "pij"], [k + "pijf"])
            i4 = idxf.rearrange("p (h f i) -> p h f i", h=8, f=2)
            ohv = oh.rearrange("p (h s i) -> p h s i", h=8, s=16)
            for f in range(2):
                pf_ = pijf[:, f * 128:(f + 1) * 128].rearrange("p (h s) -> p h s", h=8)
                for h_ in range(8):
                    self.tt(ohv[:, h_], pf_[:, h_, :].unsqueeze(2).broadcast_to([128, 16, 16]),
                            iota.unsqueeze(1).broadcast_to([128, 16, 16]), ALU.is_equal, [k + "pijf", k + "iota", k + "sc"], [k + "oh", k + "sc"])
                    self.tt(ohv[:, h_], ohv[:, h_], i4[:, h_, f, :].unsqueeze(1).broadcast_to([128, 16, 16]), ALU.mult,
                            [k + "oh", k + "idxf"], [k + "oh"])
                self.S.op("dve", lambda h, f=f: h.tensor_reduce(out=asel[:, f * 128:(f + 1) * 128], in_=oh.rearrange("p (a i) -> p a i", i=16),
                                                                 axis=AX.X, op=ALU.add), [k + "oh"], [k + "asel"])
            self.stt(eidf, asel[:, 0:128], 128.0, asel[:, 128:256], ALU.mult, ALU.add, [k + "asel"], [k + "eidf"])
            self.cp(eid, eidf, [k + "eidf"], [k + "eid"])
            t3 = tops.rearrange("p (h s) -> p h s", h=8)
            self.ts(ntop, t3[:, :, 0], -1.0, None, ALU.mult, None, [k + "tops"], [k + "ntop"])
            for h_ in range(8):
                self.act(gate[:, h_ * 16:(h_ + 1) * 16], tops[:, h_ * 16:(h_ + 1) * 16], AF.Exp, [k + "tops", k + "ntop"], [k + "gate", k + "zs"],
                         bias=ntop[:, h_:h_ + 1], accum=zs[:, h_:h_ + 1])
            self.S.op("dve", lambda h: h.reciprocal(out=zs, in_=zs), [k + "zs"], [k + "zs"])
            self.tt(gate.rearrange("p (h s) -> p h s", h=8), gate.rearrange("p (h s) -> p h s", h=8),
                    zs.unsqueeze(2).broadcast_to([128, 8, 16]), ALU.mult, [k + "gate", k + "zs"], [k + "gate"])
            psF = self.ps[:, 2560:3584]
            GS = 4
            NGRP = 128 // GS

            def grp_front(g):
                nonlocal gcount
                for j in range(GS):
                    s_ = g * GS + j
                    gb = gcount % NUV
                    gcount += 1
                    uvb_, uvk = UV[gb], k + "UV%d" % gb
                    slot_buf[s_] = (uvb_, uvk)
                    self.S.dma("pool", lambda h, uvb_=uvb_, s_=s_: h.indirect_dma_start(
                        out=uvb_, out_offset=None, in_=self.uvb[l], in_offset=bass.IndirectOffsetOnAxis(ap=eid[:, s_:s_ + 1], axis=0)),
                        [k + "eid", ("uvb", l)], [uvk])
                    self.S.op("dve", lambda h, uvb_=uvb_, s_=s_: h.scalar_tensor_tensor(
                        out=self.junkd, in0=uvb_[:, 0:1024], scalar=1.0, in1=hx2, op0=ALU.mult, op1=ALU.mult,
                        accum_out=sdot[:, s_:s_ + 1]), [uvk, k + "hx2"], [k + "sdot%d" % (g % 2)])
                sl = slice(g * GS, (g + 1) * GS)
                sk_, t1k, t2k = k + "sdot%d" % (g % 2), k + "t1_%d" % (g % 2), k + "t2_%d" % (g % 2)
                self.tt(t1[:, sl], sdot[:, sl], sdot[:, sl], ALU.mult, [sk_], [t1k])
                self.ts(t1[:, sl], t1[:, sl], 0.044715, 1.0, ALU.mult, ALU.add, [t1k], [t1k])
                self.tt(t1[:, sl], t1[:, sl], sdot[:, sl], ALU.mult, [t1k, sk_], [t1k])
                self.act(t2[:, sl], t1[:, sl], AF.Sigmoid, [t1k], [t2k], scale=1.5957691216)

            def grp_back(g):
                sl = slice(g * GS, (g + 1) * GS)
                sk_, t2k, ak = k + "sdot%d" % (g % 2), k + "t2_%d" % (g % 2), k + "actv%d" % (g % 2)
                self.tt(t2[:, sl], t2[:, sl], sdot[:, sl], ALU.mult, [t2k, sk_], [t2k])
                self.tt(actv[:, sl], t2[:, sl], gate[:, sl], ALU.mult, [t2k, k + "gate"], [ak])
                for j in range(GS):
                    s_ = g * GS + j
                    uvb_, uvk = slot_buf[s_]
                    dgb = dg[s_ % 8]
                    dgk = k + "dg%d" % (s_ % 8)
                    self.act(dgb, self.identf, AF.Copy, ["identf", ak], [dgk], scale=actv[:, s_:s_ + 1])
                    for nn in range(2):
                        self.mm(psF[:, nn * 512:(nn + 1) * 512], dgb, uvb_[:, 1024 + nn * 512:1024 + (nn + 1) * 512], s_ == 0, s_ == 127,
                                [dgk, uvk], ["ps%d" % (5 + nn)])

            slot_buf = {}
            for g in range(NGRP + 1):
                if g < NGRP:
                    grp_front(g)
                if g >= 1:
                    grp_back(g - 1)
            xw = xo[n % 2]
            xwk = k + "xo"
            self.tt(tmpf, psF, G2, ALU.mult, ["ps5", "ps6", k + "G2"], [k + "tmpf"])
            if not last:
                self.tt(xw, tmpf, xm, ALU.add, [k + "tmpf", k + "xm"], [xwk])
                self.ld(self.xres[b, i * 128:(i + 1) * 128, :], xw, [xwk], [("xres", b, i)])
            else:
                self.tt(xm, tmpf, xm, ALU.add, [k + "tmpf", k + "xm"], [k + "xm"])
                self.act(self.junk, xm, AF.Square, [k + "xm"], [k + "ss"], accum=ss)
                self.rstd(rs, ss, 1.0 / 1024, tm1, [k + "ss"], k + "rs")
                self.stt(xw, xm, rs, fn, ALU.mult, ALU.mult, [k + "xm", k + "rs", k + "fn"], [xwk])
                tok = self.ld(self.out[b, (i - 2) * 128:(i - 1) * 128, :], xw, [xwk], [("out", b, i)])
                self.final.append(tok)
        ar.release()


def _rope_tables():
    t = np.arange(2048)
    inv = 10000.0 ** (-np.arange(0, 32, 2, dtype=np.float32) / 32.0)
    row = (t // 64).astype(np.float32)[None, :] * inv[:, None]
    col = (t % 64).astype(np.float32)[None, :] * inv[:, None]
    cosT = np.zeros((64, 2048), np.float32)
    sinT = np.zeros((64, 2048), np.float32)
    for d in range(64):
        ang = row[d % 16] if d < 32 else col[d % 16]
        cosT[d] = np.cos(ang)
        sgn = -1.0 if (d % 32) < 16 else 1.0
        sinT[d] = sgn * np.sin(ang)
    return np.tile(cosT, (2, 1)), np.tile(sinT, (2, 1))


def _band_mask():
    i = np.arange(128)[:, None]
    j = np.arange(384)[None, :]
    return np.where((j >= i) & (j <= i + 256), 0.0, NEG).astype(np.float32)


def _bm_tables(rpb):
    out = np.full((5, 4, 128, 512), NEG, np.float32)
    pair_rows = {0: (0, 1), 1: (2, 3), 2: (4, 5), 3: (28, 29), 4: (30, 31)}
    qc = np.arange(64)
    win0 = np.clip(qc - 8, 0, 48)
    for cls, rows in pair_rows.items():
        for hh, r in enumerate(rows):
            r0 = min(max(r - 4, 0), 24)
            for kr in range(8):
                drow = r0 + kr - r
                kc = np.arange(64)
                dcol = np.clip(kc[None, :] - qc[:, None], -15, 15)
                ok = (kc[None, :] >= win0[:, None]) & (kc[None, :] < win0[:, None] + 16)
                for h in range(4):
                    bias = rpb[h, drow + 7][dcol + 15]
                    out[cls, h, hh * 64:(hh + 1) * 64, kr * 64:(kr + 1) * 64] = np.where(ok, bias, NEG)
    return np.ascontiguousarray(out.transpose(2, 0, 1, 3).reshape(128, 20 * 512))


def _hconst():
    j = np.arange(128)[:, None]
    i = np.arange(128)[None, :]
    same = (j // 64) == (i // 64)
    jl = j % 64
    c = np.zeros((9, 128, 128), np.float32)
    triF = same & (j <= i)
    midF = same & (jl <= 31)
    c[0] = triF
    c[1] = triF.astype(np.float32) - midF.astype(np.float32)
    c[2] = same & (j > i)
    triB = same & (j >= i)
    midB = same & (jl >= 32)
    c[3] = triB
    c[4] = triB.astype(np.float32) - midB.astype(np.float32)
    c[5] = same & (j < i)
    c[6][:, 0] = (np.arange(128) < 64)
    c[6][:, 1] = (np.arange(128) >= 64)
    return np.ascontiguousarray(c.transpose(1, 0, 2).reshape(128, 9 * 128))


_SPLITS = np.cumsum([0, 256, 128, 128, 256, 256, 256, 256, 256, 256, 256, 256, 256, 256, 256])


def _col(name):
    names = ["aq", "ak", "av", "bq", "bk", "bv", "cx", "cb", "cc", "dq", "dzf", "dzb", "di", "dg"]
    n = names.index(name)
    return np.arange(_SPLITS[n], _SPLITS[n + 1])


def _weight_layouts(w_in):
    part = np.arange(256) ^ 16
    aq, ak = _col("aq"), _col("ak")
    akd = np.concatenate([ak[0:64], ak[0:64], ak[64:128], ak[64:128]])
    fcols = np.concatenate([aq, aq[part], akd, akd[part], _col("bq"), _col("bk"), _col("dq"), _col("dzf"), _col("dzb"),
                            _col("cx"), _col("cb"), _col("cc")])
    tcols = np.concatenate([_col("av"), _col("bv"), _col("dzf"), _col("dzb"), _col("di"), _col("dg")])
    return np.ascontiguousarray(w_in[:, :, fcols]), np.ascontiguousarray(w_in[:, :, tcols])


_PROG_CACHE = {}


def _get_prog(**kw):
    key = repr(sorted(kw.items()))
    if key not in _PROG_CACHE:
        p = Prog(**kw)
        p.build()
        _PROG_CACHE[key] = p
    return _PROG_CACHE[key]


def make_in_maps(inp, n_cores=8):
    f32 = lambda a: np.ascontiguousarray(np.asarray(a, dtype=np.float32))
    x, c, ctx, c_ctx = f32(inp["x"]), f32(inp["c"]), f32(inp["ctx"]), f32(inp["c_ctx"])
    D = inp["w_mod"].shape[0]
    WF, WT = _weight_layouts(f32(inp["w_in"]))
    cosT, sinT = _rope_tables()
    conv = f32(inp["conv_w"])
    convT = np.ascontiguousarray(conv.reshape(D, 3, 2, 128).transpose(0, 3, 2, 1).reshape(D, 128, 6))
    BM = np.stack([_bm_tables(f32(inp["na_rpb"])[l]) for l in range(D)])
    lbl = f32(inp["lb_logits"])
    lblT = np.ascontiguousarray(lbl.reshape(2, 2, 128).transpose(2, 0, 1).reshape(128, 4))
    kT = np.ascontiguousarray(np.concatenate([f32(inp["peer_k1"]).transpose(0, 2, 1), f32(inp["peer_k2"]).transpose(0, 2, 1)], axis=2))
    common = {
        "w_mod": f32(inp["w_mod"]), "b_mod": f32(inp["b_mod"]), "norm_mix": f32(inp["norm_mix"]), "norm_ffn": f32(inp["norm_ffn"]),
        "WF": WF, "WT": WT, "ropec": cosT, "ropes": sinT, "convT": convT, "sink": f32(inp["attn_sink"]), "BM": BM,
        "band": _band_mask(), "lbl": lbl, "lblT": lblT, "hconst": _hconst(), "gnorm": f32(inp["group_norm"]),
        "w_out": f32(inp["w_out"]), "wq": f32(inp["peer_wq"]), "kT": kT,
        "fnorm": f32(inp["final_norm"]).reshape(1, 1024),
        "iota16": np.ascontiguousarray(np.tile(np.arange(16, dtype=np.float32)[None, :], (128, 1))),
    }
    pu, pv = f32(inp["peer_u"]), f32(inp["peer_v"])
    for l_ in range(D):
        common["uv%d" % l_] = np.ascontiguousarray(np.concatenate([pu[l_], pv[l_]], axis=1))
    maps = []
    for core in range(n_cores):
        b0 = 2 * core
        xin = np.ascontiguousarray(np.concatenate([ctx[b0:b0 + 2], x[b0:b0 + 2]], axis=1))
        c3 = np.stack([c[b0], c[b0 + 1], c_ctx], axis=1)
        cT = np.ascontiguousarray(c3.reshape(8, 128, 3).transpose(1, 0, 2).reshape(128, 24))
        m = dict(common)
        m["xin"] = xin
        m["cT"] = cT
        maps.append(m)
    return maps


def kernel(**inputs):
    prog = _get_prog(depth=2)
    maps = make_in_maps(inputs, 8)
    res = run_bass_kernel_spmd(prog.nc, maps, core_ids=list(range(8)))
    out = np.concatenate([np.asarray(r["out"], dtype=np.float32) for r in res.results], axis=0)
    return out
```

```python
from contextlib import ExitStack
import numpy as np
import concourse.bass as bass
import concourse.mybir as mybir
from concourse.bass_utils import run_bass_kernel_spmd

F32 = mybir.dt.float32
BF16 = mybir.dt.bfloat16
U32 = mybir.dt.uint32
AF = mybir.ActivationFunctionType
ALU = mybir.AluOpType
AX = mybir.AxisListType

N_DSEM = 24
N_HW_DSEM = 14
EPS = 1e-6
NEG = -1e30
TS = 2304
NTL = 18
NFC = 24
NFS = 20
NTM = 1408


class Sched:
    ENGS = ("pe", "dve", "act", "pool", "sp")

    def __init__(self, nc, same_engine_sync=True):
        self.nc = nc
        self.q = {e: [] for e in self.ENGS}
        self.n = {e: 0 for e in self.ENGS}
        self.waited = {}
        self.last_w = {}
        self.readers = {}
        self.dma_i = 0
        self.dma_sw = 0
        self.dsem_uses = [0] * N_DSEM
        self.same = same_engine_sync
        self.final_tokens = []
        self.bar = []

    def _deps(self, reads, writes):
        deps = []
        for r in reads:
            t = self.last_w.get(r)
            if t is not None:
                deps.append(t)
        for w in writes:
            t = self.last_w.get(w)
            if t is not None:
                deps.append(t)
            deps.extend(self.readers.get(w, ()))
        deps.extend(self.bar)
        return deps

    def _waits_for(self, eng, deps):
        need = {}
        for t in deps:
            if t[0] == "eng":
                _, e2, c = t
                if e2 == eng and (not self.same or eng == "pe"):
                    continue
                key = ("eng", e2)
            else:
                _, k, c = t
                key = ("dma", k)
            if self.waited.get((eng, key), 0) >= c:
                continue
            if need.get(key, 0) < c:
                need[key] = c
        for key, c in need.items():
            self.waited[(eng, key)] = c
        return list(need.items())

    def _commit(self, tok, reads, writes):
        for w in writes:
            self.last_w[w] = tok
            self.readers[w] = []
        for r in reads:
            if r in writes:
                continue
            lst = self.readers.setdefault(r, [])
            lst.append(tok)
            if len(lst) > 48:
                best = {}
                for t in lst:
                    k = (t[0], t[1])
                    if k not in best or best[k][2] < t[2]:
                        best[k] = t
                self.readers[r] = list(best.values())

    def barrier(self):
        self.bar = [("eng", e, self.n[e]) for e in self.ENGS if self.n[e]]
        self.bar += [("dma", k_, 16 * self.dsem_uses[k_]) for k_ in range(N_DSEM) if self.dsem_uses[k_]]

    def op(self, eng, fn, reads=(), writes=()):
        reads, writes = tuple(reads), tuple(writes)
        waits = self._waits_for(eng, self._deps(reads, writes))
        self.n[eng] += 1
        tok = ("eng", eng, self.n[eng])
        self.q[eng].append((waits, fn, ("eng", eng)))
        self._commit(tok, reads, writes)
        return tok

    def dma(self, eng, fn, reads=(), writes=()):
        reads, writes = tuple(reads), tuple(writes)
        if eng == "pool":
            k = N_HW_DSEM + self.dma_sw % (N_DSEM - N_HW_DSEM)
            self.dma_sw += 1
        else:
            k = self.dma_i % N_HW_DSEM
            self.dma_i += 1
        prev = self.dsem_uses[k] * 16
        self.dsem_uses[k] += 1
        deps = self._deps(reads, writes)
        if prev > 0:
            deps.append(("dma", k, prev))
        waits = self._waits_for(eng, deps)
        tok = ("dma", k, prev + 16)
        self.q[eng].append((waits, fn, ("dma", k)))
        self._commit(tok, reads, writes)
        return tok

    def finish(self, tokens):
        self.final_tokens = list(tokens)

    def emit(self, stack):
        nc = self.nc
        esem = {e: stack.enter_context(nc.semaphore("s_" + e)) for e in self.ENGS}
        dsem = [stack.enter_context(nc.semaphore("d_%d" % i)) for i in range(N_DSEM)]
        with nc.Block() as b0:
            @b0.sync
            def _(h):
                for sm in list(esem.values()) + dsem:
                    h.sem_clear(sm)
        block = stack.enter_context(nc.Block())

        def semof(key):
            return esem[key[1]] if key[0] == "eng" else dsem[key[1]]

        def run(eng_name, h):
            for waits, fn, inc in self.q[eng_name]:
                for key, c in waits:
                    h.wait_ge(semof(key), c)
                ins = fn(h)
                if inc[0] == "eng":
                    ins.then_inc(esem[inc[1]], 1)
                else:
                    ins.then_inc(dsem[inc[1]], 16)
            if eng_name == "sp":
                for k_ in range(N_DSEM):
                    if self.dsem_uses[k_]:
                        h.wait_ge(dsem[k_], 16 * self.dsem_uses[k_])
                for e_ in self.ENGS:
                    if self.n[e_]:
                        h.wait_ge(esem[e_], self.n[e_])

        @block.tensor
        def _(h):
            run("pe", h)

        @block.vector
        def _(h):
            run("dve", h)

        @block.scalar
        def _(h):
            run("act", h)

        @block.gpsimd
        def _(h):
            run("pool", h)

        @block.sync
        def _(h):
            run("sp", h)


class Arena:
    def __init__(self, nc, stack, name, kbytes):
        self.cols = kbytes * 256
        self.t = stack.enter_context(nc.sbuf_tensor(name, [128, self.cols], F32))
        self.off = 0
        self.marks = []
        self.peak = 0
        self.on_release = None

    def alloc(self, n, dtype=F32):
        bpe = 2 if dtype == BF16 else 4
        words = (n * bpe + 3) // 4
        words = (words + 7) // 8 * 8
        assert self.off + words <= self.cols, ("SBUF arena overflow", self.off, words, self.cols)
        ap = self.t[:, self.off:self.off + words]
        self.off += words
        self.peak = max(self.peak, self.off)
        if dtype != F32:
            ap = ap.bitcast(dtype)
        return ap[:, 0:n]

    def mark(self):
        self.marks.append(self.off)

    def release(self):
        self.off = self.marks.pop()
        if self.on_release is not None:
            self.on_release()


class Prog:
    def __init__(self, depth=2, phases=None, dbg=(), ntile_peer=None):
        self.depth = depth
        self.phases = phases
        self.dbg = set(dbg)
        self.ntile_peer = ntile_peer
        self.nc = bass.Bass("TRN2", target_bir_lowering=False)
        self.uid = 0

    def want(self, l, p):
        if self.phases is None:
            return True
        if isinstance(p, str):
            sub = [q for q in self.phases if q[0] == l and isinstance(q[1], str)]
            return (l, p) in self.phases if sub else (l, 2) in self.phases
        return (l, p) in self.phases

    def u(self, base):
        self.uid += 1
        return "%s_%d" % (base, self.uid)

    def din(self, name, shape, dt=F32):
        return self.nc.dram_tensor(name, list(shape), dt, kind="ExternalInput").ap()

    def dscr(self, name, shape, dt=F32):
        kind = "ExternalOutput" if name in self.dbg else "Internal"
        return self.nc.dram_tensor(name, list(shape), dt, kind=kind).ap()

    def dump(self, name, ap, shape, reads, dt=F32):
        if name not in self.dbg:
            return
        d = self.nc.dram_tensor(name, list(shape), dt, kind="ExternalOutput").ap()
        self.ld(d, ap, reads, [self.u("dump")])

    def mm(self, out, lhsT, rhs, start, stop, r, w):
        return self.S.op("pe", lambda h: h.matmul(out, lhsT=lhsT, rhs=rhs, start=start, stop=stop), r, w)

    def tr(self, out, in_, ident, r, w):
        return self.S.op("pe", lambda h: h.transpose(out=out, in_=in_, identity=ident), r, w)

    def act(self, out, in_, func, r, w, bias=None, scale=None, accum=None, eng="act"):
        kw = {}
        if bias is not None:
            kw["bias"] = bias
        if scale is not None:
            kw["scale"] = scale
        if accum is not None:
            kw["accum_out"] = accum
        return self.S.op("act", lambda h: h.activation(out=out, in_=in_, func=func, **kw), r, w)

    def tt(self, out, in0, in1, op, r, w, eng="dve"):
        return self.S.op(eng, lambda h: h.tensor_tensor(out=out, in0=in0, in1=in1, op=op), r, w)

    def ts(self, out, in0, s1, s2, op0, op1, r, w, eng="dve"):
        if op1 is None:
            return self.S.op(eng, lambda h: h.tensor_scalar(out=out, in0=in0, scalar1=s1, scalar2=None, op0=op0), r, w)
        return self.S.op(eng, lambda h: h.tensor_scalar(out=out, in0=in0, scalar1=s1, scalar2=s2, op0=op0, op1=op1), r, w)

    def stt(self, out, in0, scalar, in1, op0, op1, r, w):
        return self.S.op("dve", lambda h: h.scalar_tensor_tensor(out=out, in0=in0, scalar=scalar, in1=in1, op0=op0, op1=op1), r, w)

    def cp(self, out, in_, r, w, eng="dve"):
        if eng == "act":
            return self.S.op("act", lambda h: h.activation(out=out, in_=in_, func=AF.Copy), r, w)
        return self.S.op(eng, lambda h: h.tensor_copy(out=out, in_=in_), r, w)

    def ld(self, out, in_, r, w, q="sp"):
        return self.S.dma(q, lambda h: h.dma_start(out=out, in_=in_), r, w)

    def memset(self, ap, val, w, eng="pool"):
        return self.S.op(eng, lambda h: h.memset(ap, val), (), w)

    def build(self):
        nc = self.nc
        D = self.depth
        I = {}
        I["xin"] = self.din("xin", [2, TS, 1024])
        I["cT"] = self.din("cT", [128, 24])
        I["w_mod"] = self.din("w_mod", [D, 1024, 6144])
        I["b_mod"] = self.din("b_mod", [D, 6144])
        I["norm_mix"] = self.din("norm_mix", [D, 1024])
        I["norm_ffn"] = self.din("norm_ffn", [D, 1024])
        I["WF"] = self.din("WF", [D, 1024, NFC * 128])
        I["WT"] = self.din("WT", [D, 1024, NTM])
        I["ropec"] = self.din("ropec", [128, 2048])
        I["ropes"] = self.din("ropes", [128, 2048])
        I["convT"] = self.din("convT", [D, 128, 6])
        I["sink"] = self.din("sink", [D, 4])
        I["BM"] = self.din("BM", [D, 128, 20 * 512])
        I["band"] = self.din("band", [128, 384])
        I["lbl"] = self.din("lbl", [2, 256])
        I["lblT"] = self.din("lblT", [128, 4])
        I["hconst"] = self.din("hconst", [128, 9 * 128])
        if self.phases is None or any(p[1] == 3 for p in self.phases):
            I["gnorm"] = self.din("gnorm", [D, 1024])
            I["w_out"] = self.din("w_out", [D, 1024, 1024])
            I["wq"] = self.din("wq", [D, 1024, 2048])
            I["kT"] = self.din("kT", [D, 128, 256])
            for l_ in range(D):
                I["uv%d" % l_] = self.din("uv%d" % l_, [16384, 2048])
            I["fnorm"] = self.din("fnorm", [1, 1024])
            I["iota16"] = self.din("iota16", [128, 16])
        self.I = I
        self.out = nc.dram_tensor("out", [2, 2048, 1024], F32, kind="ExternalOutput").ap()
        self.modv = self.dscr("modv", [3, 6, 1024])
        self.PF = self.dscr("PF", [NFS * 128, 2, TS], BF16)
        self.PT = self.dscr("PT", [2, TS, NTM], BF16)
        self.ymix = self.dscr("ymix", [2, TS, 1024])
        self.xres = self.dscr("xres", [2, TS, 1024])
        self.uvb = [self.dscr("uvb%d" % l_, [16384, 2048], BF16) for l_ in range(D)]
        self.dbgq = self.dscr("dbgq", [128, 128 * 4]) if "dbgq" in self.dbg else None

        with ExitStack() as st:
            self.ar = Arena(nc, st, "arena", 190)
            self.ps = st.enter_context(nc.psum_tensor("ps", [128, 4096], F32))
            self.S = Sched(nc)
            self.ar.on_release = self.S.barrier
            self.final = []
            self.setup_consts()
            if self.phases is None or any(p[1] == 3 for p in self.phases):
                self.phase_tables()
            for l in range(D):
                last = l == D - 1
                if self.want(l, 0):
                    self.phase_mod(l)
                if self.want(l, 1):
                    self.phase_proj(l)
                if self.want(l, 2):
                    self.phase_mix(l, last)
                if self.want(l, 3):
                    self.phase_ffn(l, last)
            self.S.finish(self.final)
            self.S.emit(st)
        return nc

    def bank(self, k):
        return self.ps[:, k * 512:(k + 1) * 512]

    def setup_consts(self):
        ar = self.ar
        self.identf = ar.alloc(128)
        self.identb = ar.alloc(128, BF16)
        self.memset(self.identf, 0.0, ["identf"])
        self.S.op("pool", lambda h: h.affine_select(out=self.identf, in_=self.identf, pattern=[[-1, 128]],
                                                     compare_op=ALU.not_equal, fill=1.0, base=0, channel_multiplier=1),
                  ["identf"], ["identf"])
        self.cp(self.identb, self.identf, ["identf"], ["identb"])
        self.junk = ar.alloc(1024, BF16)
        self.junkd = ar.alloc(1024, BF16)

    def rstd(self, out, ss, inv_n, tmp, r, w):
        self.ts(tmp, ss, inv_n, EPS, ALU.mult, ALU.add, r, [w + "_t"])
        self.act(tmp, tmp, AF.Sqrt, [w + "_t"], [w + "_t"])
        self.S.op("dve", lambda h: h.reciprocal(out=out, in_=tmp), [w + "_t"], [w])

    def phase_mod(self, l):
        ar, I = self.ar, self.I
        ar.mark()
        cT = ar.alloc(24)
        scT = ar.alloc(24)
        msb = ar.alloc(6144)
        bmb = ar.alloc(6144)
        nmx = ar.alloc(1024)
        nff = ar.alloc(1024)
        wch = [ar.alloc(8 * 512) for _ in range(2)]
        k = self.u("mod")
        self.ld(cT, I["cT"], [], [k + "cT"])
        self.act(scT, cT, AF.Silu, [k + "cT"], [k + "scT"])
        self.ld(bmb[0:3, :], I["b_mod"][l:l + 1, :].partition_broadcast(3), [], [k + "bmb"])
        self.ld(nmx[0:3, :], I["norm_mix"][l:l + 1, :].partition_broadcast(3), [], [k + "nmx"])
        self.ld(nff[0:3, :], I["norm_ffn"][l:l + 1, :].partition_broadcast(3), [], [k + "nff"])
        wv = I["w_mod"][l].rearrange("(k p) n -> p k n", p=128)
        for n in range(12):
            wb = wch[n % 2]
            wk = k + "w%d" % (n % 2)
            self.ld(wb.rearrange("p (k n) -> p k n", k=8), wv[:, :, n * 512:(n + 1) * 512], [], [wk])
            pb = self.bank(n % 2)
            pk = "ps%d" % (n % 2)
            for kk in range(8):
                self.mm(pb[0:3, :], scT[:, kk * 3:(kk + 1) * 3], wb[:, kk * 512:(kk + 1) * 512],
                        kk == 0, kk == 7, [k + "scT", wk], [pk])
            self.tt(msb[0:3, n * 512:(n + 1) * 512], pb[0:3, :], bmb[0:3, n * 512:(n + 1) * 512], ALU.add,
                    [pk, k + "bmb"], [k + "msb"])
        self.dump("d_scT", scT, [128, 24], [k + "scT"])
        self.dump("d_msb", msb[0:3, :], [3, 6144], [k + "msb"])
        self.stt(msb[0:3, 1024:2048], msb[0:3, 1024:2048], 1.0, nmx[0:3, :], ALU.add, ALU.mult,
                 [k + "msb", k + "nmx"], [k + "msb"])
        self.stt(msb[0:3, 4096:5120], msb[0:3, 4096:5120], 1.0, nff[0:3, :], ALU.add, ALU.mult,
                 [k + "msb", k + "nff"], [k + "msb"])
        order = [1, 0, 2, 4, 3, 5]
        for slot, j in enumerate(order):
            self.ld(self.modv[:, slot, :], msb[0:3, j * 1024:(j + 1) * 1024], [k + "msb"], ["modv"])
        ar.release()

    def load_bc(self, dst, slot, r, key):
        self.ld(dst, self.modv[r, slot:slot + 1, :].partition_broadcast(128), ["modv"], [key])

    def phase_proj(self, l):
        ar, I = self.ar, self.I
        ar.mark()
        k = self.u("pj")
        wF = ar.alloc(8 * NFC * 128, BF16)
        wT = ar.alloc(8 * NTM, BF16)
        wFv = wF.rearrange("p (k n) -> p k n", k=8)
        wTv = wT.rearrange("p (k n) -> p k n", k=8)
        srcF = I["WF"][l].rearrange("(k p) n -> p k n", p=128)
        srcT = I["WT"][l].rearrange("(k p) n -> p k n", p=128)
        for c in range(0, NFC * 128, 512):
            self.ld(wFv[:, :, c:c + 512], srcF[:, :, c:c + 512], [], [k + "wF"], q="pool")
        for c in range(0, NTM, 352):
            self.ld(wTv[:, :, c:c + 352], srcT[:, :, c:c + 352], [], [k + "wT"], q="pool")
        cosT = ar.alloc(2048)
        sinT = ar.alloc(2048)
        self.ld(cosT, I["ropec"], [], [k + "cos"])
        self.ld(sinT, I["ropes"], [], [k + "sin"])
        A1 = ar.alloc(1024)
        B1 = ar.alloc(1024)
        xt = [ar.alloc(1024) for _ in range(2)]
        tmpf = ar.alloc(1024)
        hx = ar.alloc(1024, BF16)
        hxT = ar.alloc(1024, BF16)
        pfs = [ar.alloc(NFS * 128, BF16) for _ in range(2)]
        pts = [ar.alloc(NTM, BF16) for _ in range(2)]
        ss = ar.alloc(1)
        rs = ar.alloc(1)
        tm1 = ar.alloc(1)
        rt1 = ar.alloc(128)
        rt2 = ar.alloc(128)
        xsrc = I["xin"] if l == 0 else self.xres
        tiles = [(b, i) for b in range(2) for i in range(NTL)]

        def xkey(b, i):
            return [] if l == 0 else [("xres", b, i)]

        def loads(n):
            b, i = tiles[n]
            self.ld(xt[n % 2], xsrc[b, i * 128:(i + 1) * 128, :], xkey(b, i), [k + "xt%d" % (n % 2)])

        loads(0)
        cur_r = None
        pfv_d = self.PF.rearrange("(c p) b s -> p c b s", p=128)
        for n, (b, i) in enumerate(tiles):
            if n + 1 < len(tiles):
                loads(n + 1)
            r = 2 if i < 2 else b
            if r != cur_r:
                self.load_bc(A1, 0, r, k + "A1")
                self.load_bc(B1, 1, r, k + "B1")
                cur_r = r
            x = xt[n % 2]
            xk = k + "xt%d" % (n % 2)
            self.act(self.junk, x, AF.Square, [xk], [k + "ss"], accum=ss)
            self.rstd(rs, ss, 1.0 / 1024, tm1, [k + "ss"], k + "rs")
            self.stt(tmpf, x, rs, A1, ALU.mult, ALU.mult, [xk, k + "rs", k + "A1"], [k + "tmpf"])
            self.tt(hx, tmpf, B1, ALU.add, [k + "tmpf", k + "B1"], [k + "hx"])
            pT = self.bank(0).bitcast(BF16)
            for c in range(8):
                self.tr(pT[:, c * 128:(c + 1) * 128], hx[:, c * 128:(c + 1) * 128], self.identb,
                        [k + "hx", "identb"], ["ps0"])
            self.cp(hxT, pT, ["ps0"], [k + "hxT"], eng="act")
            hxTv = hxT.rearrange("p (c t) -> p c t", c=8)
            pf = pfs[n % 2]
            pfk = k + "pf%d" % (n % 2)
            pfv = pf.rearrange("p (c t) -> p c t", c=NFS)
            latent = i >= 2
            t0 = i * 128 - 256
            for g in range(NFC // 4):
                bk = 1 + g % 3
                pk = "ps%d" % bk
                pb = self.bank(bk)
                for j in range(4):
                    fc = g * 4 + j
                    for kk in range(8):
                        self.mm(pb[:, j * 128:(j + 1) * 128], wFv[:, kk, fc * 128:(fc + 1) * 128], hxTv[:, kk, :],
                                kk == 0, kk == 7, [k + "wF", k + "hxT"], [pk])
                if g < 2:
                    for j in range(2):
                        dst = pfv[:, 2 * g + j, :]
                        if latent:
                            self.tt(rt1, pb[:, j * 128:(j + 1) * 128], cosT[:, t0:t0 + 128], ALU.mult,
                                    [pk, k + "cos"], [k + "rt1"])
                            self.tt(rt2, pb[:, (2 + j) * 128:(3 + j) * 128], sinT[:, t0:t0 + 128], ALU.mult,
                                    [pk, k + "sin"], [k + "rt2"])
                            self.tt(dst, rt1, rt2, ALU.add, [k + "rt1", k + "rt2"], [pfk])
                        else:
                            self.cp(dst, pb[:, j * 128:(j + 1) * 128], [pk], [pfk])
                else:
                    dst = pf[:, (4 + (g - 2) * 4) * 128:(8 + (g - 2) * 4) * 128]
                    self.cp(dst, pb, [pk], [pfk], eng=("act" if g % 2 else "dve"))
            self.ld(pfv_d[:, :, b, i * 128:(i + 1) * 128], pfv, [pfk], [("PF", b, i)])
            pt = pts[n % 2]
            ptk = k + "pt%d" % (n % 2)
            for g, (c0, c1) in enumerate([(0, 512), (512, 1024), (1024, NTM)]):
                bk = 4 + g
                pk = "ps%d" % bk
                pb = self.bank(bk)
                for kk in range(8):
                    self.mm(pb[:, 0:c1 - c0], hxTv[:, kk, :], wTv[:, kk, c0:c1], kk == 0, kk == 7,
                            [k + "wT", k + "hxT"], [pk])
                self.cp(pt[:, c0:c1], pb[:, 0:c1 - c0], [pk], [ptk], eng=("act" if g % 2 else "dve"))
            self.ld(self.PT[b, i * 128:(i + 1) * 128, :], pt, [ptk], [("PT", b, i)])
        ar.release()

    def attn_unit(self, k, nq_parts, score_mms, nloc, bias_ap, bias_key, nctx, sink_col, pv_list, out_ap, out_key, W, sink_key=None):
        S_sb, P_sb, PT_sb, m, negm, rsum, es, rinv = W["S"], W["P"], W["PT"], W["m"], W["negm"], W["rsum"], W["es"], W["rinv"]
        psA, psB, psT, psO = self.bank(0), self.bank(1), self.bank(2).bitcast(BF16), self.bank(3)
        for (o, lt, rh, rd, pk) in score_mms:
            self.mm(o, lt, rh, True, True, rd, [pk])
        ntot = nloc + nctx
        if nloc:
            self.stt(S_sb[:, 0:nloc], psA[:, 0:nloc], 0.125, bias_ap, ALU.mult, ALU.add, ["ps0", bias_key], [k + "S"])
        self.act(S_sb[:, nloc:ntot], psB[:, 0:nctx], AF.Copy, ["ps1"], [k + "S"], scale=0.125)
        self.S.op("dve", lambda h: h.reduce_max(out=m, in_=S_sb[:, 0:ntot], axis=AX.X), [k + "S"], [k + "m"])
        if sink_col is not None:
            self.tt(m, m, sink_col, ALU.max, [k + "m", sink_key], [k + "m"])
        self.ts(negm, m, -1.0, None, ALU.mult, None, [k + "m"], [k + "negm"])
        self.act(P_sb[:, 0:ntot], S_sb[:, 0:ntot], AF.Exp, [k + "S", k + "negm"], [k + "P", k + "rsum"], bias=negm, accum=rsum)
        if sink_col is not None:
            self.act(es, sink_col, AF.Exp, [sink_key, k + "negm"], [k + "es"], bias=negm)
            self.tt(rsum, rsum, es, ALU.add, [k + "rsum", k + "es"], [k + "rsum"])
        self.S.op("dve", lambda h: h.reciprocal(out=rinv, in_=rsum), [k + "rsum"], [k + "rinv"])
        nch = ntot // 128
        for c in range(nch):
            self.tr(psT[:, c * 128:(c + 1) * 128], P_sb[:, c * 128:(c + 1) * 128], self.identb, [k + "P", "identb"], ["ps2"])
        self.cp(PT_sb[:, 0:ntot], psT[:, 0:ntot], ["ps2"], [k + "PT"], eng="act")
        for (p0, p1, items) in pv_list:
            for ii, (c, rhs, rd) in enumerate(items):
                self.mm(psO[p0:p1, 0:64], PT_sb[:, c * 128 + p0:c * 128 + p1], rhs, ii == 0, ii == len(items) - 1,
                        [k + "PT"] + rd, ["ps3"])
        self.ts(out_ap, psO[:, 0:64], rinv, None, ALU.mult, None, ["ps3", k + "rinv"], [out_key])

    def phase_mix(self, l, last):
        ar, I = self.ar, self.I
        ar.mark()
        k = self.u("mx")
        band = ar.alloc(384)
        self.ld(band, I["band"], [], [k + "band"])
        sinkb = ar.alloc(4)
        self.ld(sinkb, I["sink"][l:l + 1, :].partition_broadcast(128), [], [k + "sink"])
        convT = ar.alloc(6)
        self.ld(convT, I["convT"][l], [], [k + "convT"])
        hc = ar.alloc(9 * 128)
        self.ld(hc, I["hconst"], [], [k + "hc"])
        lbm_bc = ar.alloc(256)
        oml_bc = ar.alloc(256)
        lbT = ar.alloc(4)
        lbm_pp = ar.alloc(2)
        oml_pp = ar.alloc(2)
        if l == 0:
            self.memset(lbm_bc, 1e-20, [k + "lbm"])
            self.memset(oml_bc, 1.0, [k + "oml"])
            self.memset(lbm_pp, 1e-20, [k + "lbpp"])
            self.memset(oml_pp, 1.0, [k + "lbpp"])
        else:
            l0 = ar.alloc(256)
            self.ld(l0, I["lbl"][0:1, :].partition_broadcast(128), [], [k + "l0"])
            self.ld(lbm_bc, I["lbl"][1:2, :].partition_broadcast(128), [], [k + "lbm"])
            self.tt(lbm_bc, lbm_bc, l0, ALU.subtract, [k + "l0", k + "lbm"], [k + "lbm"])
            self.act(oml_bc, lbm_bc, AF.Sigmoid, [k + "lbm"], [k + "oml"], scale=-1.0)
            self.act(lbm_bc, lbm_bc, AF.Sigmoid, [k + "lbm"], [k + "lbm"])
            self.ts(lbm_bc, lbm_bc, 1e-20, None, ALU.max, None, [k + "lbm"], [k + "lbm"])
            self.ld(lbT, I["lblT"], [], [k + "lbT"])
            self.tt(lbm_pp, lbT[:, 2:4], lbT[:, 0:2], ALU.subtract, [k + "lbT"], [k + "lbpp"])
            self.act(oml_pp, lbm_pp, AF.Sigmoid, [k + "lbpp"], [k + "lbpp"], scale=-1.0)
            self.act(lbm_pp, lbm_pp, AF.Sigmoid, [k + "lbpp"], [k + "lbpp"])
            self.ts(lbm_pp, lbm_pp, 1e-20, None, ALU.max, None, [k + "lbpp"], [k + "lbpp"])
        PFv = self.PF.rearrange("(c p) b s -> p c b s", p=128)
        for b in range(2):
            pfk = [("PF", b, i) for i in range(NTL)]
            ptk = [("PT", b, i) for i in range(NTL)]
            PTv = self.PT[b].rearrange("(i p) c -> p i c", p=128)
            if self.want(l, "A") or self.want(l, "B"):
                self.mix_attn(l, last, b, k, band, sinkb, PFv, PTv, pfk, ptk)
            if self.want(l, "C"):
                self.mix_conv(l, last, b, k, convT, PFv, pfk)
            if self.want(l, "D"):
                self.mix_hgrn(l, last, b, k, hc, lbm_bc, oml_bc, lbm_pp, oml_pp, PFv, PTv, pfk, ptk)
        ar.release()

    def mix_attn(self, l, last, b, k0, band, sinkb, PFv, PTv, pfk, ptk):
        ar, I = self.ar, self.I
        ar.mark()
        k = self.u(k0 + "at")
        BM = ar.alloc(20 * 512)
        self.ld(BM, I["BM"][l], [], [k + "BM"])
        BMv = BM.rearrange("p (c n) -> p c n", c=20)
        q = {}
        for nm, c0 in (("qA", 0), ("kA", 2), ("qB", 4), ("kB", 6)):
            t = ar.alloc(2 * TS, BF16)
            tv = t.rearrange("p (c s) -> p c s", c=2)
            self.ld(tv, PFv[:, c0:c0 + 2, b, :], pfk, [k + nm])
            q[nm] = tv
        vA = ar.alloc(NTL * 128, BF16).rearrange("p (i c) -> p i c", i=NTL)
        vB = ar.alloc(NTL * 256, BF16).rearrange("p (i c) -> p i c", i=NTL)
        vBs = ar.alloc(15 * 256, BF16).rearrange("p (i c) -> p i c", i=15)
        self.ld(vA, PTv[:, :, 0:128], ptk, [k + "vA"])
        self.ld(vB, PTv[:, :, 128:384], ptk, [k + "vB"])
        PTs = self.PT[b, 320:320 + 15 * 128, :].rearrange("(i p) c -> p i c", p=128)
        self.ld(vBs, PTs[:, :, 128:384], ptk, [k + "vBs"])
        W = dict(S=ar.alloc(768), P=ar.alloc(768, BF16), PT=ar.alloc(768, BF16), m=ar.alloc(1), negm=ar.alloc(1),
                 rsum=ar.alloc(1), es=ar.alloc(1), rinv=ar.alloc(1))
        yo = [ar.alloc(256) for _ in range(2)]
        psA, psB = self.bank(0), self.bank(1)
        cnt = 0
        if self.want(l, "A"):
            units = [("lat", n) for n in range(16)]
            if not last:
                units += [("ctx", n) for n in range(2)]
            for kind, n in units:
                y = yo[cnt % 2]
                yk = k + "yo%d" % (cnt % 2)
                cnt += 1
                for h in range(4):
                    fc, pb = h // 2, (h % 2) * 64
                    if kind == "lat":
                        t_lo, t_hi = max(0, 128 * n - 128), min(2048, 128 * n + 256)
                        nk = t_hi - t_lo
                        moff = t_lo - (128 * n - 128)
                        qcols = slice(256 + 128 * n, 256 + 128 * n + 128)
                    else:
                        t_lo = nk = moff = 0
                        qcols = slice(128 * n, 128 * n + 128)
                    lt = q["qA"][pb:pb + 64, fc, qcols]
                    sm = []
                    if nk:
                        sm.append((psA[:, 0:nk], lt, q["kA"][pb:pb + 64, fc, 256 + t_lo:256 + t_lo + nk], [k + "qA", k + "kA"], "ps0"))
                    sm.append((psB[:, 0:256], lt, q["kA"][pb:pb + 64, fc, 0:256], [k + "qA", k + "kA"], "ps1"))
                    items = []
                    for c in range(nk // 128):
                        items.append((c, vA[:, 2 + t_lo // 128 + c, fc * 64:(fc + 1) * 64], [k + "vA"]))
                    for c in range(2):
                        items.append((nk // 128 + c, vA[:, c, fc * 64:(fc + 1) * 64], [k + "vA"]))
                    self.attn_unit(k, 1, sm, nk, band[:, moff:moff + nk] if nk else None, k0 + "band",
                                   256, sinkb[:, h:h + 1], [(0, 128, items)], y[:, h * 64:(h + 1) * 64], yk, W, sink_key=k0 + "sink")
                s0 = 256 + 128 * n if kind == "lat" else 128 * n
                self.ld(self.ymix[b, s0:s0 + 128, 0:256], y, [yk], [("ymix", b, s0 // 128, 0)])
        if self.want(l, "B"):
            units = [("lat", rp) for rp in range(16)]
            if not last:
                units += [("ctx", n) for n in range(2)]
            for kind, rp in units:
                y = yo[cnt % 2]
                yk = k + "yo%d" % (cnt % 2)
                cnt += 1
                for h in range(4):
                    fc, pb = h // 2, (h % 2) * 64
                    sm = []
                    pv = []
                    if kind == "lat":
                        cls = 0 if rp == 0 else 1 if rp == 1 else 3 if rp == 14 else 4 if rp == 15 else 2
                        for hh in range(2):
                            r = 2 * rp + hh
                            r0 = min(max(r - 4, 0), 24)
                            lt = q["qB"][pb:pb + 64, fc, 256 + 64 * r:256 + 64 * r + 64]
                            sm.append((psA[hh * 64:(hh + 1) * 64, 0:512], lt, q["kB"][pb:pb + 64, fc, 256 + 64 * r0:256 + 64 * r0 + 512],
                                       [k + "qB", k + "kB"], "ps0"))
                            sm.append((psB[hh * 64:(hh + 1) * 64, 0:256], lt, q["kB"][pb:pb + 64, fc, 0:256], [k + "qB", k + "kB"], "ps1"))
                            items = []
                            for c in range(4):
                                if r0 % 2 == 0:
                                    items.append((c, vB[:, 2 + r0 // 2 + c, h * 64:(h + 1) * 64], [k + "vB"]))
                                else:
                                    items.append((c, vBs[:, (r0 - 1) // 2 + c, h * 64:(h + 1) * 64], [k + "vBs"]))
                            for c in range(2):
                                items.append((4 + c, vB[:, c, h * 64:(h + 1) * 64], [k + "vB"]))
                            pv.append((hh * 64, hh * 64 + 64, items))
                        self.attn_unit(k, 2, sm, 512, BMv[:, cls * 4 + h, :], k + "BM", 256, None, pv,
                                       y[:, h * 64:(h + 1) * 64], yk, W)
                    else:
                        lt = q["qB"][pb:pb + 64, fc, 128 * rp:128 * rp + 128]
                        sm.append((psB[:, 0:256], lt, q["kB"][pb:pb + 64, fc, 0:256], [k + "qB", k + "kB"], "ps1"))
                        items = [(c, vB[:, c, h * 64:(h + 1) * 64], [k + "vB"]) for c in range(2)]
                        self.attn_unit(k, 1, sm, 0, None, None, 256, None, [(0, 128, items)], y[:, h * 64:(h + 1) * 64], yk, W)
                s0 = 256 + 128 * rp if kind == "lat" else 128 * rp
                self.ld(self.ymix[b, s0:s0 + 128, 256:512], y, [yk], [("ymix", b, s0 // 128, 1)])
        ar.release()

    def mix_conv(self, l, last, b, k0, convT, PFv, pfk):
        ar = self.ar
        ar.mark()
        k = self.u(k0 + "cv")
        cin = ar.alloc(6 * TS, BF16).rearrange("p (c s) -> p c s", c=6)
        self.ld(cin, PFv[:, 14:20, b, :], pfk, [k + "cin"])
        u = ar.alloc(TS)
        acc = ar.alloc(TS)
        ycs = ar.alloc(NTL * 256).rearrange("p (i c) -> p i c", i=NTL)
        rngs = [(256, TS)] if last else [(0, 256), (256, TS)]
        for dt in range(2):
            self.tt(u, cin[:, 4 + dt, :], cin[:, 0 + dt, :], ALU.mult, [k + "cin"], [k + "u"])
            for (a, e) in rngs:
                w0, w1, w2 = (convT[:, dt * 3 + j:dt * 3 + j + 1] for j in range(3))
                self.ts(acc[:, a:e], u[:, a:e], w1, None, ALU.mult, None, [k + "u", k0 + "convT"], [k + "acc"])
                self.stt(acc[:, a + 1:e], u[:, a:e - 1], w0, acc[:, a + 1:e], ALU.mult, ALU.add, [k + "u", k + "acc"], [k + "acc"])
                self.stt(acc[:, a:e - 1], u[:, a + 1:e], w2, acc[:, a:e - 1], ALU.mult, ALU.add, [k + "u", k + "acc"], [k + "acc"])
            self.tt(acc, acc, cin[:, 2 + dt, :], ALU.mult, [k + "acc", k + "cin"], [k + "acc"])
            for i in range(0 if not last else 2, NTL):
                bk = 4 + i % 2
                pk = "ps%d" % bk
                self.tr(self.bank(bk)[:, 0:128], acc[:, i * 128:(i + 1) * 128], self.identf, [k + "acc", "identf"], [pk])
                self.cp(ycs[:, i, dt * 128:(dt + 1) * 128], self.bank(bk)[:, 0:128], [pk], [k + "ycs"], eng=("act" if i % 2 else "dve"))
        i0 = 0 if not last else 2
        dst = self.ymix[b].rearrange("(i p) c -> p i c", p=128)
        self.ld(dst[:, i0:NTL, 512:768], ycs[:, i0:NTL, :], [k + "ycs"], [("ymix", b, i, 2) for i in range(i0, NTL)])
        ar.release()

    def mix_hgrn(self, l, last, b, k0, hc, lbm_bc, oml_bc, lbm_pp, oml_pp, PFv, PTv, pfk, ptk):
        ar = self.ar
        ar.mark()
        k = self.u(k0 + "hg")
        hcv = hc.rearrange("p (c n) -> p c n", c=9)
        qz = ar.alloc(6 * TS, BF16).rearrange("p (c s) -> p c s", c=6)
        self.ld(qz, PFv[:, 8:14, b, :], pfk, [k + "qz"])
        od = ar.alloc(NTL * 256).rearrange("p (i c) -> p i c", i=NTL)
        Sst = ar.alloc(128)
        Sbf = ar.alloc(128, BF16)
        tok = [ar.alloc(1024, BF16) for _ in range(2)]
        sg = ar.alloc(256)
        lf = ar.alloc(256)
        kft = ar.alloc(256)
        kfT = ar.alloc(256)
        E13 = ar.alloc(512)
        E2T = ar.alloc(256)
        E4 = ar.alloc(256)
        dec = ar.alloc(4)
        qtil = ar.alloc(256, BF16)
        ktil = ar.alloc(256, BF16)
        qhat = ar.alloc(256, BF16)
        khat = ar.alloc(256, BF16)
        attTs = [ar.alloc(128, BF16) for _ in range(2)]
        gsl = ar.alloc(256)
        yd = [ar.alloc(256) for _ in range(2)]
        for d in range(2):
            order = list(range(NTL)) if d == 0 else [1, 0] + list(range(NTL - 1, 1, -1))
            cTri, cX, cE = hcv[:, 3 * d + 0, :], hcv[:, 3 * d + 1, :], hcv[:, 3 * d + 2, :]
            csel = hcv[:, 6, 0:2]
            attT = attTs[d]
            self.memset(attT, 0.0, [k + "attT0"])
            self.memset(Sst, 0.0, [k + "S"])
            self.memset(Sbf, 0.0, [k + "Sbf"])

            def loads(n):
                i = order[n]
                self.ld(tok[n % 2], PTv[:, i, 384:1408], [ptk[i]], [k + "tok%d" % (n % 2)])

            loads(0)
            for n, i in enumerate(order):
                if n + 1 < len(order):
                    loads(n + 1)
                tk = tok[n % 2]
                tkk = k + "tok%d" % (n % 2)
                zt = tk[:, d * 256:(d + 1) * 256]
                it = tk[:, 512:768]
                cols = slice(i * 128, (i + 1) * 128)
                self.act(sg, zt, AF.Sigmoid, [tkk], [k + "sg"])
                self.tt(sg, sg, oml_bc, ALU.mult, [k + "sg", k0 + "oml"], [k + "sg"])
                self.tt(sg, sg, lbm_bc, ALU.add, [k + "sg", k0 + "lbm"], [k + "sg"])
                self.act(lf, sg, AF.Ln, [k + "sg"], [k + "lf"])
                self.act(kft, zt, AF.Sigmoid, [tkk], [k + "kft"], scale=-1.0)
                self.tt(kft, kft, oml_bc, ALU.mult, [k + "kft", k0 + "oml"], [k + "kft"])
                for dt in range(2):
                    self.act(kfT[:, dt * 128:(dt + 1) * 128], qz[:, 2 + 2 * d + dt, cols], AF.Sigmoid, [k + "qz"], [k + "kfT"], scale=-1.0)
                    self.ts(kfT[:, dt * 128:(dt + 1) * 128], kfT[:, dt * 128:(dt + 1) * 128], oml_pp[:, dt:dt + 1], None, ALU.mult, None,
                            [k + "kfT", k0 + "lbpp"], [k + "kfT"])
                b0, b1 = self.bank(0), self.bank(1)
                for dt in range(2):
                    lt = lf[:, dt * 128:(dt + 1) * 128]
                    self.mm(b0[:, dt * 128:(dt + 1) * 128], lt, cTri, True, True, [k + "lf", k0 + "hc"], ["ps0"])
                    self.mm(b0[:, 256 + dt * 128:256 + (dt + 1) * 128], lt, cX, True, True, [k + "lf", k0 + "hc"], ["ps0"])
                    self.mm(b1[:, 256 + dt * 2:256 + dt * 2 + 2], lt, csel, True, True, [k + "lf", k0 + "hc"], ["ps1"])
                self.mm(b1[:, 0:256], cE, lf, True, True, [k + "lf", k0 + "hc"], ["ps1"])
                self.act(E13, b0, AF.Exp, ["ps0"], [k + "E13"])
                self.act(E2T, b0[:, 256:512], AF.Exp, ["ps0"], [k + "E2T"], scale=-1.0)
                self.act(E4, b1[:, 0:256], AF.Exp, ["ps1"], [k + "E4"])
                self.act(dec, b1[:, 256:260], AF.Exp, ["ps1"], [k + "dec"])
                qTv = qz[:, 0:2, cols]
                self.tt(qtil.rearrange("p (c t) -> p c t", c=2), qTv, E13[:, 256:512].rearrange("p (c t) -> p c t", c=2), ALU.mult,
                        [k + "qz", k + "E13"], [k + "qtil"])
                self.tt(qhat.rearrange("p (c t) -> p c t", c=2), qTv, E13[:, 0:256].rearrange("p (c t) -> p c t", c=2), ALU.mult,
                        [k + "qz", k + "E13"], [k + "qhat"])
                self.tt(ktil, kfT, E2T, ALU.mult, [k + "kfT", k + "E2T"], [k + "ktil"])
                self.tt(khat, kft, E4, ALU.mult, [k + "kft", k + "E4"], [k + "khat"])
                psO = self.bank(4)
                corder = (0, 1) if d == 0 else (1, 0)
                for dt in range(2):
                    for hp in range(2):
                        h = dt * 2 + hp
                        pb = hp * 64
                        bk = 2 + h % 2
                        pk = "ps%d" % bk
                        self.mm(self.bank(bk)[:, 0:128], ktil[pb:pb + 64, dt * 128:(dt + 1) * 128], qtil[pb:pb + 64, dt * 128:(dt + 1) * 128],
                                True, True, [k + "ktil", k + "qtil"], [pk])
                        self.S.op("dve", lambda h, bk=bk, attT=attT, cTri=cTri: h.copy_predicated(
                            out=attT, mask=cTri.bitcast(U32), data=self.bank(bk)[:, 0:128]), [pk, k0 + "hc", k + "attT0"], [k + "attT"])
                        osl = psO[:, h * 64:(h + 1) * 64]
                        self.mm(osl, attT, it[:, h * 64:(h + 1) * 64], True, False, [k + "attT", tkk], ["ps4"])
                        for ci, c in enumerate(corder):
                            self.mm(psO[c * 64:(c + 1) * 64, h * 64:(h + 1) * 64],
                                    qhat[pb:pb + 64, dt * 128 + c * 64:dt * 128 + c * 64 + 64],
                                    Sbf[pb:pb + 64, dt * 64:(dt + 1) * 64], False, True,
                                    [k + "qhat", k + "Sbf", k + "Sbf%d_%d" % (dt, hp)], ["ps4"])
                            if ci == 0:
                                self.state_update(k, d, dt, hp, c, khat, it, dec, Sst, Sbf, tkk)
                        self.state_update(k, d, dt, hp, corder[1], khat, it, dec, Sst, Sbf, tkk)
                if d == 0:
                    self.cp(od[:, i, :], psO[:, 0:256], ["ps4"], [k + "od%d" % i], eng="act")
                else:
                    self.tt(od[:, i, :], psO[:, 0:256], od[:, i, :], ALU.add, ["ps4", k + "od%d" % i], [k + "od%d" % i])
                    if not (last and i < 2):
                        y = yd[n % 2]
                        yk = k + "yd%d" % (n % 2)
                        self.act(gsl, tk[:, 768:1024], AF.Silu, [tkk], [k + "gsl"])
                        self.tt(y, od[:, i, :], gsl, ALU.mult, [k + "od%d" % i, k + "gsl"], [yk])
                        self.ld(self.ymix[b, i * 128:(i + 1) * 128, 768:1024], y, [yk], [("ymix", b, i, 3)])
        ar.release()

    def state_update(self, k, d, dt, hp, c, khat, it, dec, Sst, Sbf, tkk):
        h = dt * 2 + hp
        pb = hp * 64
        bk = 5 + (h % 2)
        pk = "ps%d" % bk
        psD = self.bank(bk)
        self.mm(psD[pb:pb + 64, 0:64], khat[c * 64:(c + 1) * 64, h * 64:(h + 1) * 64], it[c * 64:(c + 1) * 64, h * 64:(h + 1) * 64],
                True, True, [k + "khat", tkk], [pk])
        sk = k + "S%d_%d" % (dt, hp)
        self.stt(Sst[pb:pb + 64, dt * 64:(dt + 1) * 64], Sst[pb:pb + 64, dt * 64:(dt + 1) * 64], dec[pb:pb + 64, dt * 2 + c:dt * 2 + c + 1],
                 psD[pb:pb + 64, 0:64], ALU.mult, ALU.add, [k + "S", sk, k + "dec", pk], [sk])
        self.cp(Sbf[pb:pb + 64, dt * 64:(dt + 1) * 64], Sst[pb:pb + 64, dt * 64:(dt + 1) * 64], [sk, k + "S", k + "Sbf"], [k + "Sbf%d_%d" % (dt, hp)], eng="act")

    def phase_tables(self):
        ar, I = self.ar, self.I
        ar.mark()
        k = self.u("tb")
        RJ = 4
        stg = [ar.alloc(RJ * 2048, BF16) for _ in range(3)]
        n = 0
        for l in range(self.depth):
            if not any(self.want(l, 3) for _ in (0,)):
                continue
            src = I["uv%d" % l].rearrange("(p j) n -> p j n", p=128)
            dst = self.uvb[l].rearrange("(p j) n -> p j n", p=128)
            for j0 in range(0, 128, RJ):
                sb = stg[n % 3]
                sk = k + "stg%d" % (n % 3)
                n += 1
                self.ld(sb.rearrange("p (j n) -> p j n", j=RJ), src[:, j0:j0 + RJ, :], [], [sk], q="pool")
                self.ld(dst[:, j0:j0 + RJ, :], sb.rearrange("p (j n) -> p j n", j=RJ), [sk], [("uvb", l)])
        ar.release()

    def phase_ffn(self, l, last):
        ar, I = self.ar, self.I
        ar.mark()
        k = self.u("ff")
        wout = ar.alloc(8 * 1024, BF16).rearrange("p (k n) -> p k n", k=8)
        wq = ar.alloc(8 * 2048, BF16).rearrange("p (k n) -> p k n", k=8)
        kT = ar.alloc(256, BF16)
        src = I["w_out"][l].rearrange("(k p) n -> p k n", p=128)
        for c in range(0, 1024, 512):
            self.ld(wout[:, :, c:c + 512], src[:, :, c:c + 512], [], [k + "wout"], q="pool")
        src = I["wq"][l].rearrange("(k p) n -> p k n", p=128)
        for c in range(0, 2048, 512):
            self.ld(wq[:, :, c:c + 512], src[:, :, c:c + 512], [], [k + "wq"], q="pool")
        self.ld(kT, I["kT"][l], [], [k + "kT"], q="pool")
        gn = ar.alloc(1024)
        self.ld(gn, I["gnorm"][l:l + 1, :].partition_broadcast(128), [], [k + "gn"])
        iota = ar.alloc(16)
        self.ld(iota, I["iota16"], [], [k + "iota"])
        fn = None
        if last:
            fn = ar.alloc(1024)
            self.ld(fn, I["fnorm"].partition_broadcast(128), [], [k + "fn"])
        G1, A2, B2, G2 = (ar.alloc(1024) for _ in range(4))
        ym = [ar.alloc(1024)] * 2
        xt = [ar.alloc(1024)] * 2
        yn = ar.alloc(1024, BF16)
        ynT = ar.alloc(1024, BF16)
        xm = ar.alloc(1024)
        tmpf = ar.alloc(1024)
        hx2 = ar.alloc(1024)
        hx2b = yn
        hx2T = ynT
        qT = ar.alloc(16 * 128, BF16)
        sc = ar.alloc(16 * 128)
        work = ar.alloc(256)
        vals = ar.alloc(256)
        idxu = ar.alloc(256, U32)
        idxf = ar.alloc(256)
        cand = sc
        tops = ar.alloc(128)
        posu = ar.alloc(128, U32)
        pij = ar.alloc(256, U32)
        pijf = ar.alloc(256)
        oh = sc
        asel = ar.alloc(256)
        eidf = ar.alloc(128)
        eid = ar.alloc(128, U32)
        gate = ar.alloc(128)
        ntop = ar.alloc(8)
        zs = ar.alloc(8)
        sdot = ar.alloc(128)
        actv = ar.alloc(128)
        sgt = ar.alloc(128)
        junks = [self.junkd] + [ar.alloc(1024, BF16) for _ in range(3)]
        t1 = ar.alloc(128)
        t2 = ar.alloc(128)
        ssg = ar.alloc(4)
        rg = ar.alloc(4)
        tg = ar.alloc(4)
        ss = ar.alloc(1)
        rs = ar.alloc(1)
        tm1 = ar.alloc(1)
        NUV = 12
        UV = [ar.alloc(2048, BF16) for _ in range(NUV)]
        dg = [ar.alloc(128, BF16) for _ in range(8)]
        xo = [ar.alloc(1024)] * 2
        xsrc = I["xin"] if l == 0 else self.xres
        tiles = [(b, i) for b in range(2) for i in range(NTL) if not (last and i < 2)]
        if self.ntile_peer is not None:
            tiles = tiles[:self.ntile_peer]

        def loads(n):
            b, i = tiles[n]
            self.ld(ym[n % 2], self.ymix[b, i * 128:(i + 1) * 128, :], [("ymix", b, i, j) for j in range(4)], [k + "ym"])
            self.ld(xt[n % 2], xsrc[b, i * 128:(i + 1) * 128, :], [] if l == 0 else [("xres", b, i)], [k + "xt"])

        loads(0)
        cur_r = None
        gcount = 0
        for n, (b, i) in enumerate(tiles):
            r = 2 if i < 2 else b
            if r != cur_r:
                for dst, slot, nm in ((G1, 2, "G1"), (A2, 3, "A2"), (B2, 4, "B2"), (G2, 5, "G2")):
                    self.load_bc(dst, slot, r, k + nm)
                cur_r = r
            y, x = ym[n % 2], xt[n % 2]
            yk, xk = k + "ym", k + "xt"
            for g in range(4):
                self.act(self.junk[:, 0:256], y[:, g * 256:(g + 1) * 256], AF.Square, [yk], [k + "ssg"], accum=ssg[:, g:g + 1])
            self.rstd(rg, ssg, 1.0 / 256, tg, [k + "ssg"], k + "rg")
            for g in range(4):
                self.stt(yn[:, g * 256:(g + 1) * 256], y[:, g * 256:(g + 1) * 256], rg[:, g:g + 1], gn[:, g * 256:(g + 1) * 256],
                         ALU.mult, ALU.mult, [yk, k + "rg", k + "gn"], [k + "yn"])
            pT = self.bank(0).bitcast(BF16)
            for c in range(8):
                self.tr(pT[:, c * 128:(c + 1) * 128], yn[:, c * 128:(c + 1) * 128], self.identb, [k + "yn", "identb"], ["ps0"])
            self.cp(ynT, pT, ["ps0"], [k + "ynT"], eng="act")
            ynTv = ynT.rearrange("p (c t) -> p c t", c=8)
            for nn in range(2):
                pk = "ps%d" % (1 + nn)
                for kk in range(8):
                    self.mm(self.bank(1 + nn), ynTv[:, kk, :], wout[:, kk, nn * 512:(nn + 1) * 512], kk == 0, kk == 7,
                            [k + "ynT", k + "wout"], [pk])
            psY = self.ps[:, 512:1536]
            self.tt(tmpf, psY, G1, ALU.mult, ["ps1", "ps2", k + "G1"], [k + "tmpf"])
            self.tt(xm, tmpf, x, ALU.add, [k + "tmpf", xk], [k + "xm"])
            if n + 1 < len(tiles):
                loads(n + 1)
            self.act(self.junk, xm, AF.Square, [k + "xm"], [k + "ss"], accum=ss)
            self.rstd(rs, ss, 1.0 / 1024, tm1, [k + "ss"], k + "rs")
            self.stt(tmpf, xm, rs, A2, ALU.mult, ALU.mult, [k + "xm", k + "rs", k + "A2"], [k + "tmpf"])
            self.tt(hx2, tmpf, B2, ALU.add, [k + "tmpf", k + "B2"], [k + "hx2"])
            self.cp(hx2b, hx2, [k + "hx2"], [k + "yn"], eng="act")
            for c in range(8):
                self.tr(pT[:, c * 128:(c + 1) * 128], hx2b[:, c * 128:(c + 1) * 128], self.identb, [k + "yn", "identb"], ["ps0"])
            self.cp(hx2T, pT, ["ps0"], [k + "ynT"], eng="act")
            hx2Tv = hx2T.rearrange("p (c t) -> p c t", c=8)
            for g in range(4):
                bk = 3 + g % 2
                pk = "ps%d" % bk
                for j in range(4):
                    qc = g * 4 + j
                    for kk in range(8):
                        self.mm(self.bank(bk)[:, j * 128:(j + 1) * 128], wq[:, kk, qc * 128:(qc + 1) * 128], hx2Tv[:, kk, :],
                                kk == 0, kk == 7, [k + "wq", k + "ynT"], [pk])
                self.cp(qT[:, g * 512:(g + 1) * 512], self.bank(bk), [pk], [k + "qT"], eng=("act" if g % 2 else "dve"))
            for g in range(4):
                bk = 3 + g % 2
                pk = "ps%d" % bk
                for j in range(4):
                    qc = g * 4 + j
                    self.mm(self.bank(bk)[:, j * 128:(j + 1) * 128], qT[:, qc * 128:(qc + 1) * 128], kT[:, (qc % 2) * 128:(qc % 2 + 1) * 128],
                            True, True, [k + "qT", k + "kT"], [pk])
                self.cp(sc[:, g * 512:(g + 1) * 512], self.bank(bk), [pk], [k + "sc"], eng=("act" if g % 2 else "dve"))
            for qc in range(16):
                s_ = sc[:, qc * 128:(qc + 1) * 128]
                v_ = vals[:, qc * 16:(qc + 1) * 16]
                i_ = idxu[:, qc * 16:(qc + 1) * 16]
                self.S.op("dve", lambda h, s_=s_, v_=v_: h.max(out=v_[:, 0:8], in_=s_), [k + "sc"], [k + "vals"])
                self.S.op("dve", lambda h, s_=s_, v_=v_, i_=i_: h.max_index(out=i_[:, 0:8], in_max=v_[:, 0:8], in_values=s_), [k + "sc", k + "vals"], [k + "idxu"])
                self.S.op("dve", lambda h, s_=s_, v_=v_: h.match_replace(out=work[:, 0:128], in_to_replace=v_[:, 0:8], in_values=s_, imm_value=NEG),
                          [k + "sc", k + "vals"], [k + "work"])
                self.S.op("dve", lambda h, v_=v_: h.max(out=v_[:, 8:16], in_=work[:, 0:128]), [k + "work"], [k + "vals"])
                self.S.op("dve", lambda h, v_=v_, i_=i_: h.max_index(out=i_[:, 8:16], in_max=v_[:, 8:16], in_values=work[:, 0:128]),
                          [k + "work", k + "vals"], [k + "idxu"])
            self.cp(idxf, idxu, [k + "idxu"], [k + "idxf"])
            v4 = vals.rearrange("p (h f i) -> p h f i", h=8, f=2)
            candv = cand.rearrange("p (h i j) -> p h i j", h=8, i=16)
            for h_ in range(8):
                self.tt(candv[:, h_], v4[:, h_, 0, :].unsqueeze(2).broadcast_to([128, 16, 16]),
                        v4[:, h_, 1, :].unsqueeze(1).broadcast_to([128, 16, 16]), ALU.add, [k + "vals", k + "sc"], [k + "cand", k + "sc"])
            for h_ in range(8):
                c_ = cand[:, h_ * 256:(h_ + 1) * 256]
                t_ = tops[:, h_ * 16:(h_ + 1) * 16]
                p_ = posu[:, h_ * 16:(h_ + 1) * 16]
                self.S.op("dve", lambda h, c_=c_, t_=t_: h.max(out=t_[:, 0:8], in_=c_), [k + "cand"], [k + "tops"])
                self.S.op("dve", lambda h, c_=c_, t_=t_, p_=p_: h.max_index(out=p_[:, 0:8], in_max=t_[:, 0:8], in_values=c_), [k + "cand", k + "tops"], [k + "posu"])
                self.S.op("dve", lambda h, c_=c_, t_=t_: h.match_replace(out=work, in_to_replace=t_[:, 0:8], in_values=c_, imm_value=NEG),
                          [k + "cand", k + "tops"], [k + "work"])
                self.S.op("dve", lambda h, t_=t_: h.max(out=t_[:, 8:16], in_=work), [k + "work"], [k + "tops"])
                self.S.op("dve", lambda h, t_=t_, p_=p_: h.max_index(out=p_[:, 8:16], in_max=t_[:, 8:16], in_values=work), [k + "work", k + "tops"], [k + "posu"])
            self.S.op("dve", lambda h: h.tensor_single_scalar(out=pij[:, 0:128], in_=posu, scalar=4, op=ALU.logical_shift_right), [k + "posu"], [k + "pij"])
            self.S.op("dve", lambda h: h.tensor_single_scalar(out=pij[:, 128:256], in_=posu, scalar=15, op=ALU.bitwise_and), [k + "posu"], [k + "pij"])
            self.cp(pijf, pij, [k + "pij"], [k + "pijf"])
            i4 = idxf.rearrange("p (h f i) -> p h f i", h=8, f=2)
            ohv = oh.rearrange("p (h s i) -> p h s i", h=8, s=16)
            for f in range(2):
                pf_ = pijf[:, f * 128:(f + 1) * 128].rearrange("p (h s) -> p h s", h=8)
                for h_ in range(8):
                    self.tt(ohv[:, h_], pf_[:, h_, :].unsqueeze(2).broadcast_to([128, 16, 16]),
                            iota.unsqueeze(1).broadcast_to([128, 16, 16]), ALU.is_equal, [k + "pijf", k + "iota", k + "sc"], [k + "oh", k + "sc"])
                    self.tt(ohv[:, h_], ohv[:, h_], i4[:, h_, f, :].unsqueeze(1).broadcast_to([128, 16, 16]), ALU.mult,
                            [k + "oh", k + "idxf"], [k + "oh"])
                self.S.op("dve", lambda h, f=f: h.tensor_reduce(out=asel[:, f * 128:(f + 1) * 128], in_=oh.rearrange("p (a i) -> p a i", i=16),
                                                                 axis=AX.X, op=ALU.add), [k + "oh"], [k + "asel"])
            self.stt(eidf, asel[:, 0:128], 128.0, asel[:, 128:256], ALU.mult, ALU.add, [k + "asel"], [k + "eidf"])
            self.cp(eid, eidf, [k + "eidf"], [k + "eid"])
            t3 = tops.rearrange("p (h s) -> p h s", h=8)
            self.ts(ntop, t3[:, :, 0], -1.0, None, ALU.mult, None, [k + "tops"], [k + "ntop"])
            for h_ in range(8):
                self.act(gate[:, h_ * 16:(h_ + 1) * 16], tops[:, h_ * 16:(h_ + 1) * 16], AF.Exp, [k + "tops", k + "ntop"], [k + "gate", k + "zs"],
                         bias=ntop[:, h_:h_ + 1], accum=zs[:, h_:h_ + 1])
            self.S.op("dve", lambda h: h.reciprocal(out=zs, in_=zs), [k + "zs"], [k + "zs"])
            self.tt(gate.rearrange("p (h s) -> p h s", h=8), gate.rearrange("p (h s) -> p h s", h=8),
                    zs.unsqueeze(2).broadcast_to([128, 8, 16]), ALU.mult, [k + "gate", k + "zs"], [k + "gate"])
            psF = self.ps[:, 2560:3584]
            GS = 4
            NGRP = 128 // GS

            def grp_front(g):
                nonlocal gcount
                for j in range(GS):
                    s_ = g * GS + j
                    gb = gcount % NUV
                    gcount += 1
                    uvb_, uvk = UV[gb], k + "UV%d" % gb
                    slot_buf[s_] = (uvb_, uvk)
                    self.S.dma("pool", lambda h, uvb_=uvb_, s_=s_: h.indirect_dma_start(
                        out=uvb_, out_offset=None, in_=self.uvb[l], in_offset=bass.IndirectOffsetOnAxis(ap=eid[:, s_:s_ + 1], axis=0)),
                        [k + "eid", ("uvb", l)], [uvk])
                    self.S.op("dve", lambda h, uvb_=uvb_, s_=s_, jb=junks[j]: h.scalar_tensor_tensor(
                        out=jb, in0=uvb_[:, 0:1024], scalar=1.0, in1=hx2, op0=ALU.mult, op1=ALU.mult,
                        accum_out=sdot[:, s_:s_ + 1]), [uvk, k + "hx2"], [k + "sd%d_%d" % (g % 2, j), k + "junk%d" % j])
                sl = slice(g * GS, (g + 1) * GS)
                sdk = [k + "sd%d_%d" % (g % 2, j) for j in range(GS)]
                t1k, t2k, sgk = k + "t1_%d" % (g % 2), k + "t2_%d" % (g % 2), k + "sg_%d" % (g % 2)
                self.stt(t1[:, sl], sdot[:, sl], 0.044715, sdot[:, sl], ALU.mult, ALU.mult, sdk, [t1k])
                self.stt(t1[:, sl], t1[:, sl], 1.0, sdot[:, sl], ALU.add, ALU.mult, [t1k] + sdk, [t1k])
                self.tt(sgt[:, sl], sdot[:, sl], gate[:, sl], ALU.mult, sdk + [k + "gate"], [sgk])
                self.act(t2[:, sl], t1[:, sl], AF.Sigmoid, [t1k], [t2k], scale=1.5957691216)

            def grp_back(g):
                sl = slice(g * GS, (g + 1) * GS)
                t2k, sgk, ak = k + "t2_%d" % (g % 2), k + "sg_%d" % (g % 2), k + "actv%d" % (g % 2)
                self.tt(actv[:, sl], t2[:, sl], sgt[:, sl], ALU.mult, [t2k, sgk], [ak])
                for j in range(GS):
                    s_ = g * GS + j
                    uvb_, uvk = slot_buf[s_]
                    dgb = dg[s_ % 8]
                    dgk = k + "dg%d" % (s_ % 8)
                    self.act(dgb, self.identf, AF.Copy, ["identf", ak], [dgk], scale=actv[:, s_:s_ + 1])
                    for nn in range(2):
                        self.mm(psF[:, nn * 512:(nn + 1) * 512], dgb, uvb_[:, 1024 + nn * 512:1024 + (nn + 1) * 512], s_ == 0, s_ == 127,
                                [dgk, uvk], ["ps%d" % (5 + nn)])

            slot_buf = {}
            for g in range(NGRP + 1):
                if g < NGRP:
                    grp_front(g)
                if g >= 1:
                    grp_back(g - 1)
            xw = xo[n % 2]
            xwk = k + "xo"
            self.tt(tmpf, psF, G2, ALU.mult, ["ps5", "ps6", k + "G2"], [k + "tmpf"])
            if not last:
                self.tt(xw, tmpf, xm, ALU.add, [k + "tmpf", k + "xm"], [xwk])
                self.ld(self.xres[b, i * 128:(i + 1) * 128, :], xw, [xwk], [("xres", b, i)])
            else:
                self.tt(xm, tmpf, xm, ALU.add, [k + "tmpf", k + "xm"], [k + "xm"])
                self.act(self.junk, xm, AF.Square, [k + "xm"], [k + "ss"], accum=ss)
                self.rstd(rs, ss, 1.0 / 1024, tm1, [k + "ss"], k + "rs")
                self.stt(xw, xm, rs, fn, ALU.mult, ALU.mult, [k + "xm", k + "rs", k + "fn"], [xwk])
                tok = self.ld(self.out[b, (i - 2) * 128:(i - 1) * 128, :], xw, [xwk], [("out", b, i)])
                self.final.append(tok)
        ar.release()


def _rope_tables():
    t = np.arange(2048)
    inv = 10000.0 ** (-np.arange(0, 32, 2, dtype=np.float32) / 32.0)
    row = (t // 64).astype(np.float32)[None, :] * inv[:, None]
    col = (t % 64).astype(np.float32)[None, :] * inv[:, None]
    cosT = np.zeros((64, 2048), np.float32)
    sinT = np.zeros((64, 2048), np.float32)
    for d in range(64):
        ang = row[d % 16] if d < 32 else col[d % 16]
        cosT[d] = np.cos(ang)
        sgn = -1.0 if (d % 32) < 16 else 1.0
        sinT[d] = sgn * np.sin(ang)
    return np.tile(cosT, (2, 1)), np.tile(sinT, (2, 1))


def _band_mask():
    i = np.arange(128)[:, None]
    j = np.arange(384)[None, :]
    return np.where((j >= i) & (j <= i + 256), 0.0, NEG).astype(np.float32)


def _bm_tables(rpb):
    out = np.full((5, 4, 128, 512), NEG, np.float32)
    pair_rows = {0: (0, 1), 1: (2, 3), 2: (4, 5), 3: (28, 29), 4: (30, 31)}
    qc = np.arange(64)
    win0 = np.clip(qc - 8, 0, 48)
    for cls, rows in pair_rows.items():
        for hh, r in enumerate(rows):
            r0 = min(max(r - 4, 0), 24)
            for kr in range(8):
                drow = r0 + kr - r
                kc = np.arange(64)
                dcol = np.clip(kc[None, :] - qc[:, None], -15, 15)
                ok = (kc[None, :] >= win0[:, None]) & (kc[None, :] < win0[:, None] + 16)
                for h in range(4):
                    bias = rpb[h, drow + 7][dcol + 15]
                    out[cls, h, hh * 64:(hh + 1) * 64, kr * 64:(kr + 1) * 64] = np.where(ok, bias, NEG)
    return np.ascontiguousarray(out.transpose(2, 0, 1, 3).reshape(128, 20 * 512))


def _hconst():
    j = np.arange(128)[:, None]
    i = np.arange(128)[None, :]
    same = (j // 64) == (i // 64)
    jl = j % 64
    c = np.zeros((9, 128, 128), np.float32)
    triF = same & (j <= i)
    midF = same & (jl <= 31)
    c[0] = triF
    c[1] = triF.astype(np.float32) - midF.astype(np.float32)
    c[2] = same & (j > i)
    triB = same & (j >= i)
    midB = same & (jl >= 32)
    c[3] = triB
    c[4] = triB.astype(np.float32) - midB.astype(np.float32)
    c[5] = same & (j < i)
    c[6][:, 0] = (np.arange(128) < 64)
    c[6][:, 1] = (np.arange(128) >= 64)
    return np.ascontiguousarray(c.transpose(1, 0, 2).reshape(128, 9 * 128))


_SPLITS = np.cumsum([0, 256, 128, 128, 256, 256, 256, 256, 256, 256, 256, 256, 256, 256, 256])


def _col(name):
    names = ["aq", "ak", "av", "bq", "bk", "bv", "cx", "cb", "cc", "dq", "dzf", "dzb", "di", "dg"]
    n = names.index(name)
    return np.arange(_SPLITS[n], _SPLITS[n + 1])


def _weight_layouts(w_in):
    part = np.arange(256) ^ 16
    aq, ak = _col("aq"), _col("ak")
    akd = np.concatenate([ak[0:64], ak[0:64], ak[64:128], ak[64:128]])
    fcols = np.concatenate([aq, aq[part], akd, akd[part], _col("bq"), _col("bk"), _col("dq"), _col("dzf"), _col("dzb"),
                            _col("cx"), _col("cb"), _col("cc")])
    tcols = np.concatenate([_col("av"), _col("bv"), _col("dzf"), _col("dzb"), _col("di"), _col("dg")])
    return np.ascontiguousarray(w_in[:, :, fcols]), np.ascontiguousarray(w_in[:, :, tcols])


_PROG_CACHE = {}


def _get_prog(**kw):
    key = repr(sorted(kw.items()))
    if key not in _PROG_CACHE:
        p = Prog(**kw)
        p.build()
        _PROG_CACHE[key] = p
    return _PROG_CACHE[key]


def make_in_maps(inp, n_cores=8):
    f32 = lambda a: np.ascontiguousarray(np.asarray(a, dtype=np.float32))
    x, c, ctx, c_ctx = f32(inp["x"]), f32(inp["c"]), f32(inp["ctx"]), f32(inp["c_ctx"])
    D = inp["w_mod"].shape[0]
    WF, WT = _weight_layouts(f32(inp["w_in"]))
    cosT, sinT = _rope_tables()
    conv = f32(inp["conv_w"])
    convT = np.ascontiguousarray(conv.reshape(D, 3, 2, 128).transpose(0, 3, 2, 1).reshape(D, 128, 6))
    BM = np.stack([_bm_tables(f32(inp["na_rpb"])[l]) for l in range(D)])
    lbl = f32(inp["lb_logits"])
    lblT = np.ascontiguousarray(lbl.reshape(2, 2, 128).transpose(2, 0, 1).reshape(128, 4))
    kT = np.ascontiguousarray(np.concatenate([f32(inp["peer_k1"]).transpose(0, 2, 1), f32(inp["peer_k2"]).transpose(0, 2, 1)], axis=2))
    common = {
        "w_mod": f32(inp["w_mod"]), "b_mod": f32(inp["b_mod"]), "norm_mix": f32(inp["norm_mix"]), "norm_ffn": f32(inp["norm_ffn"]),
        "WF": WF, "WT": WT, "ropec": cosT, "ropes": sinT, "convT": convT, "sink": f32(inp["attn_sink"]), "BM": BM,
        "band": _band_mask(), "lbl": lbl, "lblT": lblT, "hconst": _hconst(), "gnorm": f32(inp["group_norm"]),
        "w_out": f32(inp["w_out"]), "wq": f32(inp["peer_wq"]), "kT": kT,
        "fnorm": f32(inp["final_norm"]).reshape(1, 1024),
        "iota16": np.ascontiguousarray(np.tile(np.arange(16, dtype=np.float32)[None, :], (128, 1))),
    }
    pu, pv = f32(inp["peer_u"]), f32(inp["peer_v"])
    for l_ in range(D):
        common["uv%d" % l_] = np.ascontiguousarray(np.concatenate([pu[l_], pv[l_]], axis=1))
    maps = []
    for core in range(n_cores):
        b0 = 2 * core
        xin = np.ascontiguousarray(np.concatenate([ctx[b0:b0 + 2], x[b0:b0 + 2]], axis=1))
        c3 = np.stack([c[b0], c[b0 + 1], c_ctx], axis=1)
        cT = np.ascontiguousarray(c3.reshape(8, 128, 3).transpose(1, 0, 2).reshape(128, 24))
        m = dict(common)
        m["xin"] = xin
        m["cT"] = cT
        maps.append(m)
    return maps


def kernel(**inputs):
    prog = _get_prog(depth=2)
    maps = make_in_maps(inputs, 8)
    res = run_bass_kernel_spmd(prog.nc, maps, core_ids=list(range(8)))
    out = np.concatenate([np.asarray(r["out"], dtype=np.float32) for r in res.results], axis=0)
    return out
```
